# Optimizing a Trainium2 kernel written in Bass

```python
import jax, jax.numpy as jnp
from jax import lax
import numpy as np

D_MODEL = 1024
BATCH = 4
SEQ = 4096
DEPTH = 1

D_MIX = D_MODEL
HG_WIDTH = D_MIX // 2
HG_HEADS = 4
HG_DK = HG_WIDTH // HG_HEADS
HG_DV = HG_WIDTH // HG_HEADS
HG_CHUNK = 64
POOL_WIDTH = D_MIX - HG_WIDTH
POOL_WINDOWS = (2, 4, 8, 16)
POOL_GROUPS = len(POOL_WINDOWS)
POOL_CG = POOL_WIDTH // POOL_GROUPS
IN_COLS = 4 * HG_WIDTH + POOL_WIDTH
N_EXPERTS = 64
TOP_K = 8
N_GROUP = 8
TOPK_GROUP = 4
D_EXPERT = 256
ROUTED_SCALE = 2.5
MOE_BLOCK = 128
LN_EPS = 1e-5
RMS_EPS = 1e-6
DEEPNORM_ALPHA = (2 * DEPTH) ** 0.25
DEEPNORM_BETA = (8 * DEPTH) ** -0.25

kernel_name = "hymba_hgrn2_pool_moe_deepnorm"


def layer_norm(x, g, b):
    xf = x.astype(jnp.float32)
    mu = xf.mean(-1, keepdims=True)
    var = jnp.square(xf - mu).mean(-1, keepdims=True)
    return ((xf - mu) * lax.rsqrt(var + LN_EPS) * g + b).astype(x.dtype)


def hgrn2_chunked(q, k, v, logf):
    B, S, H, DK = q.shape
    DV = v.shape[-1]
    NC = S // HG_CHUNK

    def to_chunks(t):
        return t.reshape(B, NC, HG_CHUNK, H, t.shape[-1]).transpose(1, 0, 3, 2, 4)

    qc, kc, vc, fc = (to_chunks(t) for t in (q, k, v, logf))
    causal = jnp.tril(jnp.ones((HG_CHUNK, HG_CHUNK), dtype=bool))[:, :, None]

    def step(state, inp):
        qb, kb, vb, fb = inp
        b = jnp.cumsum(fb, axis=2)
        o_inter = jnp.einsum('bhtk,bhkv->bhtv', qb * jnp.exp(b), state)
        diff = b[:, :, :, None, :] - b[:, :, None, :, :]
        decay = jnp.where(causal, jnp.exp(jnp.minimum(diff, 0.0)), 0.0)
        scores = jnp.einsum('bhtk,bhsk,bhtsk->bhts', qb, kb, decay)
        o_intra = jnp.einsum('bhts,bhsv->bhtv', scores, vb)
        b_last = b[:, :, -1:, :]
        k_dec = kb * jnp.exp(b_last - b)
        new_state = jnp.exp(b_last[:, :, 0, :])[..., None] * state + jnp.einsum('bhsk,bhsv->bhkv', k_dec, vb)
        return new_state, o_inter + o_intra

    s0 = jnp.zeros((B, H, DK, DV), jnp.float32)
    _, oc = lax.scan(step, s0, (qc, kc, vc, fc))
    return oc.transpose(1, 0, 3, 2, 4).reshape(B, S, H, DV)


def multiscale_pool(u, w_pool, pool_scale):
    B, S = u.shape[0], u.shape[1]
    uf = u.astype(jnp.float32)
    c = jnp.cumsum(uf, axis=1)
    pos = jnp.arange(1, S + 1, dtype=jnp.float32)
    outs = []
    for gi, w in enumerate(POOL_WINDOWS):
        cg = c[:, :, gi]
        prev = jnp.pad(cg, ((0, 0), (w, 0), (0, 0)))[:, :S]
        cnt = jnp.minimum(pos, float(w))[None, :, None]
        outs.append((cg - prev) / cnt - uf[:, :, gi])
    p = jnp.stack(outs, axis=2)
    p = jnp.einsum('bsgc,gcd->bsgd', p, w_pool.astype(jnp.float32))
    return (p.reshape(B, S, -1) * pool_scale).astype(u.dtype)


def hybrid_mixer(h, lb, w_in, hg_norm_g, w_pool, pool_scale, w_out):
    B, S, _ = h.shape
    proj = h @ w_in
    q, f_pre, i_v, g_out, u = jnp.split(proj, [HG_WIDTH, 2 * HG_WIDTH, 3 * HG_WIDTH, 4 * HG_WIDTH], axis=-1)
    f = lb + (1.0 - lb) * jax.nn.sigmoid(f_pre.astype(jnp.float32))
    k = 1.0 - f
    logf = jnp.log(f)
    qh = jax.nn.silu(q.astype(jnp.float32))
    shp = (B, S, HG_HEADS, HG_DK)
    o = hgrn2_chunked(qh.reshape(shp), k.reshape(shp), i_v.astype(jnp.float32).reshape(B, S, HG_HEADS, HG_DV), logf.reshape(shp))
    o = o * lax.rsqrt(jnp.mean(jnp.square(o), axis=-1, keepdims=True) + RMS_EPS)
    o = o.reshape(B, S, HG_WIDTH) * hg_norm_g * jax.nn.silu(g_out.astype(jnp.float32))
    p = multiscale_pool(u.reshape(B, S, POOL_GROUPS, POOL_CG), w_pool, pool_scale)
    mix = jnp.concatenate([o.astype(h.dtype), p], axis=-1)
    return mix @ w_out


def moe_ffn(h, w_router, router_bias, w_gate, w_up, w_down, ws_gate, ws_up, ws_down):
    B, S, D = h.shape
    T = B * S
    A = T * TOP_K
    NB = -(-(A + N_EXPERTS * (MOE_BLOCK - 1)) // MOE_BLOCK)
    xt = h.reshape(T, D)
    scores = jax.nn.sigmoid((xt @ w_router).astype(jnp.float32))
    sel = scores + router_bias.astype(jnp.float32)
    grp = sel.reshape(T, N_GROUP, N_EXPERTS // N_GROUP)
    grp_score = lax.top_k(grp, 2)[0].sum(-1)
    _, gidx = lax.top_k(grp_score, TOPK_GROUP)
    gmask = jax.nn.one_hot(gidx, N_GROUP, dtype=jnp.int32).sum(1) > 0
    emask = jnp.repeat(gmask, N_EXPERTS // N_GROUP, axis=1)
    _, eidx = lax.top_k(jnp.where(emask, sel, -jnp.inf), TOP_K)
    wts = jnp.take_along_axis(scores, eidx, axis=-1)
    wts = wts / wts.sum(-1, keepdims=True) * ROUTED_SCALE
    flat_e = eidx.reshape(A).astype(jnp.int32)
    flat_tok = jnp.repeat(jnp.arange(T, dtype=jnp.int32), TOP_K)
    flat_w = wts.reshape(A)
    order = jnp.argsort(flat_e, stable=True)
    se, stok, sw = flat_e[order], flat_tok[order], flat_w[order]
    counts = jnp.bincount(flat_e, length=N_EXPERTS)
    padded = ((counts + MOE_BLOCK - 1) // MOE_BLOCK) * MOE_BLOCK
    start = jnp.cumsum(counts) - counts
    pend = jnp.cumsum(padded)
    pstart = pend - padded
    dest = pstart[se] + (jnp.arange(A, dtype=jnp.int32) - start[se])
    tok_of_slot = jnp.full((NB * MOE_BLOCK,), T, dtype=jnp.int32).at[dest].set(stok)
    xpad = jnp.concatenate([xt, jnp.zeros((1, D), xt.dtype)], axis=0)
    xin = xpad[tok_of_slot].reshape(NB, MOE_BLOCK, D)
    blk_e = jnp.clip(jnp.searchsorted(pend, jnp.arange(NB) * MOE_BLOCK, side='right'), 0, N_EXPERTS - 1)

    def block_ffn(args):
        xb, e = args
        return (jax.nn.silu(xb @ w_gate[e]) * (xb @ w_up[e])) @ w_down[e]

    yb = lax.map(block_ffn, (xin, blk_e)).reshape(NB * MOE_BLOCK, D)
    y_assign = yb[dest] * sw[:, None].astype(yb.dtype)
    routed = jax.ops.segment_sum(y_assign, stok, num_segments=T)
    shared = (jax.nn.silu(xt @ ws_gate) * (xt @ ws_up)) @ ws_down
    return (routed + shared).reshape(B, S, D)


def setup_inputs(seed: int = 0) -> dict:
    key = jax.random.key(seed)
    ks = jax.random.split(key, 20)
    n = jax.random.normal
    f32 = jnp.float32
    col_scale = jnp.concatenate([
        jnp.ones((2 * HG_WIDTH,), f32), jnp.full((HG_WIDTH,), DEEPNORM_BETA, f32),
        jnp.ones((HG_WIDTH,), f32), jnp.full((POOL_WIDTH,), DEEPNORM_BETA, f32)])
    return {
        "x": n(ks[0], (BATCH, SEQ, D_MODEL), f32),
        "w_in": n(ks[1], (DEPTH, D_MODEL, IN_COLS), f32) * D_MODEL ** -0.5 * col_scale,
        "hg_lb_logits": n(ks[2], (DEPTH + 1, HG_WIDTH), f32) * 0.5,
        "hg_norm_g": 1.0 + 0.02 * n(ks[3], (DEPTH, HG_WIDTH), f32),
        "w_pool": n(ks[4], (DEPTH, POOL_GROUPS, POOL_CG, POOL_CG), f32) * POOL_CG ** -0.5 * DEEPNORM_BETA,
        "pool_scale": 1.0 + 0.02 * n(ks[5], (DEPTH, POOL_WIDTH), f32),
        "w_out": n(ks[6], (DEPTH, D_MIX, D_MODEL), f32) * D_MIX ** -0.5 * DEEPNORM_BETA,
        "ln1_g": 1.0 + 0.02 * n(ks[7], (DEPTH, D_MODEL), f32),
        "ln1_b": 0.02 * n(ks[8], (DEPTH, D_MODEL), f32),
        "w_router": n(ks[9], (DEPTH, D_MODEL, N_EXPERTS), f32) * D_MODEL ** -0.5,
        "router_bias": 0.01 * n(ks[10], (DEPTH, N_EXPERTS), f32),
        "w_gate": n(ks[11], (DEPTH, N_EXPERTS, D_MODEL, D_EXPERT), f32) * D_MODEL ** -0.5 * DEEPNORM_BETA,
        "w_up": n(ks[12], (DEPTH, N_EXPERTS, D_MODEL, D_EXPERT), f32) * D_MODEL ** -0.5 * DEEPNORM_BETA,
        "w_down": n(ks[13], (DEPTH, N_EXPERTS, D_EXPERT, D_MODEL), f32) * D_EXPERT ** -0.5 * DEEPNORM_BETA,
        "ws_gate": n(ks[14], (DEPTH, D_MODEL, D_EXPERT), f32) * D_MODEL ** -0.5 * DEEPNORM_BETA,
        "ws_up": n(ks[15], (DEPTH, D_MODEL, D_EXPERT), f32) * D_MODEL ** -0.5 * DEEPNORM_BETA,
        "ws_down": n(ks[16], (DEPTH, D_EXPERT, D_MODEL), f32) * D_EXPERT ** -0.5 * DEEPNORM_BETA,
        "ln2_g": 1.0 + 0.02 * n(ks[17], (DEPTH, D_MODEL), f32),
        "ln2_b": 0.02 * n(ks[18], (DEPTH, D_MODEL), f32),
    }


def reference(x, w_in, hg_lb_logits, hg_norm_g, w_pool, pool_scale, w_out, ln1_g, ln1_b,
              w_router, router_bias, w_gate, w_up, w_down, ws_gate, ws_up, ws_down, ln2_g, ln2_b):
    lower_bounds = jnp.cumsum(jax.nn.softmax(hg_lb_logits.astype(jnp.float32), axis=0), axis=0)
    h = x
    for layer in range(DEPTH):
        mix = hybrid_mixer(h, lower_bounds[layer], w_in[layer], hg_norm_g[layer], w_pool[layer],
                           pool_scale[layer], w_out[layer])
        h = layer_norm(DEEPNORM_ALPHA * h + mix, ln1_g[layer], ln1_b[layer])
        ffn = moe_ffn(h, w_router[layer], router_bias[layer], w_gate[layer], w_up[layer], w_down[layer],
                      ws_gate[layer], ws_up[layer], ws_down[layer])
        h = layer_norm(DEEPNORM_ALPHA * h + ffn, ln2_g[layer], ln2_b[layer])
    return h
```

```python
import numpy as np
import ml_dtypes
from contextlib import ExitStack
import concourse.bass as bass
import concourse.mybir as mybir
from concourse.bass_utils import run_bass_kernel_spmd

F32 = mybir.dt.float32
BF16 = mybir.dt.bfloat16
AF = mybir.ActivationFunctionType
ALU = mybir.AluOpType
AX = mybir.AxisListType

SEM_ROLL = 6000


class Tok:
    __slots__ = ("sem", "val", "eng")

    def __init__(self, sem, val, eng):
        self.sem, self.val, self.eng = sem, val, eng


class Buf:
    __slots__ = ("name", "w", "r")

    def __init__(self, name):
        self.name, self.w, self.r = name, None, []


class DSem:
    def __init__(self, prog, name):
        self.sem = prog.nc.alloc_semaphore(name)
        self.count = 0
        prog.dsems.append(self)


class Eng:
    def __init__(self, prog, name, is_pe=False, strict=False):
        self.prog, self.name, self.is_pe, self.strict = prog, name, is_pe, strict
        self.items = []
        self.waited = {}
        self.nsem = 0
        self.sem = None
        self.count = 0
        self.pend_r, self.pend_w = [], []
        self._roll()

    def _roll(self):
        self.sem = self.prog.nc.alloc_semaphore(f"c_{self.name}_{self.nsem}")
        self.nsem += 1
        self.count = 0

    def _waits(self, reads, writes):
        deps = []
        for b in reads:
            if b.w is not None:
                deps.append((b.w, True))
        for b in writes:
            if b.w is not None:
                deps.append((b.w, False))
            for t in b.r:
                deps.append((t, False))
        waits = []
        for t, raw in deps:
            if t.eng is self and (self.is_pe or (not raw and not self.strict)):
                continue
            k = id(t.sem)
            if self.waited.get(k, 0) >= t.val:
                continue
            self.waited[k] = t.val
            waits.append((t.sem, t.val))
        return waits

    def op(self, fn, reads=(), writes=(), inc=True):
        reads, writes = list(reads), list(writes)
        waits = self._waits(reads, writes)
        if not inc:
            self.items.append((waits, fn, None, 0))
            self.pend_r.extend(reads)
            self.pend_w.extend(writes)
            return None
        if self.count >= SEM_ROLL:
            self._roll()
        self.count += 1
        tok = Tok(self.sem, self.count, self)
        self.items.append((waits, fn, self.sem, 1))
        for b in reads + self.pend_r:
            b.r.append(tok)
        for b in writes + self.pend_w:
            b.w = tok
            b.r = []
        self.pend_r, self.pend_w = [], []
        return tok

    def dma(self, fn, dsem, reads=(), writes=()):
        reads, writes = list(reads), list(writes)
        waits = self._waits(reads, writes)
        dsem.count += 16
        tok = Tok(dsem.sem, dsem.count, None)
        self.items.append((waits, fn, dsem.sem, 16))
        for b in reads:
            b.r.append(tok)
        for b in writes:
            b.w = tok
            b.r = []
        return tok

    def wait_tok(self, tok):
        k = id(tok.sem)
        if self.waited.get(k, 0) >= tok.val:
            return
        self.waited[k] = tok.val
        self.items.append(([(tok.sem, tok.val)], None, None, 0))

    def replay(self, eng):
        for waits, fn, sem, n in self.items:
            for s, v in waits:
                eng.wait_ge(s, v)
            if fn is None:
                continue
            ins = fn(eng)
            if sem is not None:
                ins.then_inc(sem, n)


class Prog:
    def __init__(self, nc):
        self.nc = nc
        self.dsems = []
        self.pe = Eng(self, "pe", is_pe=True)
        self.act = Eng(self, "act", strict=True)
        self.dve = Eng(self, "dve", strict=True)
        self.pool = Eng(self, "pool", strict=True)
        self.sp = Eng(self, "sp")
        self.engs = [self.pe, self.act, self.dve, self.pool, self.sp]
        self.regcache = {}

    def bc_reg(self, eng, val):
        if val not in self.regcache:
            self.regcache[val] = eng.to_reg(val)
        return self.regcache[val]

    def barrier(self):
        toks = []
        for e in self.engs:
            assert not e.pend_r and not e.pend_w
            if e.count > 0:
                toks.append(Tok(e.sem, e.count, e))
        for d in self.dsems:
            if d.count > 0:
                toks.append(Tok(d.sem, d.count, None))
        for e in self.engs:
            for t in toks:
                if t.eng is e:
                    continue
                e.wait_tok(t)

    def emit(self):
        with self.nc.Block() as block:
            block.tensor(self.pe.replay)
            block.scalar(self.act.replay)
            block.vector(self.dve.replay)
            block.gpsimd(self.pool.replay)
            block.sync(self.sp.replay)
        for e in self.engs:
            e.items = []
        self.regcache = {}


D = 1024
NLOC = 4096
NMAIN = 2048
BLK = 512
NBLK = 8
ALPHA = float(2 ** 0.25)
LN_EPS = 1e-5
RMS_EPS = 1e-6
NEXP = 64
WINS = (2, 4, 8, 16)
CAP = 384
NROWS = NEXP * CAP
I32 = mybir.dt.int32
U32 = mybir.dt.uint32


def build(debug=None, n_exp=NEXP):
    nc = bass.Bass("TRN2", target_bir_lowering=False)

    def din(name, shape, dt=F32):
        return nc.dram_tensor(name, list(shape), dt, kind="ExternalInput").ap()

    def dout(name, shape, dt=F32):
        return nc.dram_tensor(name, list(shape), dt, kind="ExternalOutput").ap()

    xloc = din("xloc", [NLOC, D])
    w_in = din("w_in", [D, 2560])
    lb_logits = din("hg_lb_logits", [2, 512])
    hg_norm_g = din("hg_norm_g", [512])
    w_pool = din("w_pool", [4, 128, 128])
    pool_scale = din("pool_scale", [512])
    w_out = din("w_out", [D, D])
    ln1_g = din("ln1_g", [D]); ln1_b = din("ln1_b", [D])
    w_router = din("w_router", [D, 64])
    router_bias = din("router_bias", [64])
    if debug is None:
        w_gate = din("w_gate", [64, D, 256]); w_up = din("w_up", [64, D, 256]); w_down = din("w_down", [64, 256, D])
        ws_gate = din("ws_gate", [D, 256]); ws_up = din("ws_up", [D, 256]); ws_down = din("ws_down", [256, D])
        ln2_g = din("ln2_g", [D]); ln2_b = din("ln2_b", [D])
    c_identb = din("c_identb", [128, 128], BF16)
    c_identf = din("c_identf", [128, 128])
    c_cmask = din("c_cmask", [64, 64])
    c_rmask = din("c_rmask", [128, 512])
    c_invcnt = din("c_invcnt", [128, 4, 16])
    c_tri = din("c_tri", [128, 128], BF16)
    c_iota = din("c_iota", [128, 64])
    c_ecoff = din("c_ecoff", [128, 64])
    xg_dram = nc.dram_tensor("xg_dram", [NROWS, D], BF16, kind="Internal")
    y_dram = nc.dram_tensor("y_dram", [NROWS, D], BF16, kind="Internal")
    out = dout("out", [NMAIN, D])
    dbg = {}
    if debug == "mixer":
        dbg["mixT"] = dout("dbg_mixT", [128, 8, NMAIN], BF16)
    if debug in ("p4", "p4a", "p4b"):
        dbg["h1"] = dout("dbg_h1", [NMAIN, D])
        dbg["gw"] = dout("dbg_gw", [NMAIN, 64])

    P = Prog(nc)
    pe, act, dve, pool, sp = P.pe, P.act, P.dve, P.pool, P.sp
    glob = ExitStack()

    def sbt(es, name, shape, dt):
        return es.enter_context(nc.sbuf_tensor(name, list(shape), dt))

    def A(out_, in_, func, reads, writes, **kw):
        return act.op(lambda e: e.activation(out=out_, in_=in_, func=func, **kw), reads, writes)

    def TT(E, out_, in0, in1, op, reads, writes):
        return E.op(lambda e: e.tensor_tensor(out=out_, in0=in0, in1=in1, op=op), reads, writes)

    def TS(E, out_, in0, s1, s2, op0, op1, reads, writes, **kw):
        return E.op(lambda e: e.tensor_scalar(out=out_, in0=in0, scalar1=s1, scalar2=s2, op0=op0, op1=op1, **kw), reads, writes)

    def STT(out_, in0, scalar, in1, op0, op1, reads, writes, **kw):
        return dve.op(lambda e: e.scalar_tensor_tensor(out=out_, in0=in0, scalar=scalar, in1=in1, op0=op0, op1=op1, **kw), reads, writes)

    def CP(E, out_, in_, reads, writes):
        if E is act:
            return act.op(lambda e: e.activation(out=out_, in_=in_, func=AF.Copy), reads, writes)
        return E.op(lambda e: e.tensor_copy(out=out_, in_=in_), reads, writes)

    def MM(out_, lhsT, rhs, start, stop, reads, writes, inc):
        return pe.op(lambda e: e.matmul(out_, lhsT=lhsT, rhs=rhs, start=start, stop=stop), reads, writes, inc=inc)

    def TR(out_, in_, ident, reads, writes, inc):
        return pe.op(lambda e: e.transpose(out=out_, in_=in_, identity=ident), reads, writes, inc=inc)

    _bufsem = {}

    def DMA(E, out_, in_, dsem, reads=(), writes=(), **kw):
        reads, writes = list(reads), list(writes)
        key = writes[0] if writes else None
        if key is not None:
            if id(key) not in _bufsem:
                _bufsem[id(key)] = DSem(P, "dq_" + key.name)
            dsem = _bufsem[id(key)]
        return E.dma(lambda e: e.dma_start(out=out_, in_=in_, **kw), dsem, reads, writes)

    with glob:
        pb = [glob.enter_context(nc.psum_tensor(f"pb{i}", [128, 512], F32)) for i in range(8)]
        BP = [Buf(f"pb{i}") for i in range(8)]

        identb = sbt(glob, "identb", [128, 128], BF16); B_identb = Buf("identb")
        onesb = sbt(glob, "onesb", [128, 128], BF16); B_onesb = Buf("onesb")
        cmask = sbt(glob, "cmask", [64, 64], F32); B_cmask = Buf("cmask")
        rmask = sbt(glob, "rmask", [128, 512], F32); B_rmask = Buf("rmask")
        invcnt = sbt(glob, "invcnt", [128, 4, 16], F32); B_invcnt = Buf("invcnt")
        lbl = sbt(glob, "lbl", [128, 2, 4], F32); B_lbl = Buf("lbl")
        lbv = sbt(glob, "lbv", [128, 4], F32); B_lbv = Buf("lbv")
        omlv = sbt(glob, "omlv", [128, 4], F32); B_omlv = Buf("omlv")
        hgn = sbt(glob, "hgn", [128, 4], F32); B_hgn = Buf("hgn")
        pscale = sbt(glob, "pscale", [128, 4], F32); B_pscale = Buf("pscale")
        gw_all = sbt(glob, "gw_all", [128, 16, 64], F32); B_gw = [Buf(f"gw{i}") for i in range(16)]
        w8_all = sbt(glob, "w8_all", [128, 16, 8], F32); B_w8 = [Buf(f"w8_{i}") for i in range(16)]
        dest8 = sbt(glob, "dest8", [128, 128], I32); B_d8 = [Buf(f"d8_{i}") for i in range(16)]
        tri = sbt(glob, "tri", [128, 128], BF16); B_tri = Buf("tri")
        iota64 = sbt(glob, "iota64", [128, 64], F32); B_iota = Buf("iota64")
        ecoff = sbt(glob, "ecoff", [128, 64], F32); B_ecoff = Buf("ecoff")
        B_xg = Buf("xg_dram"); B_yd = Buf("y_dram")
        mixT = sbt(glob, "mixT", [128, 8, NMAIN], BF16)
        B_mix = [[Buf(f"mix{j}_{n}") for n in range(4)] for j in range(8)]

        ds_c = DSem(P, "ds_const")
        DMA(sp, identb[:], c_identb, ds_c, writes=[B_identb])
        DMA(sp, cmask[:], c_cmask, ds_c, writes=[B_cmask])
        DMA(sp, rmask[:], c_rmask, ds_c, writes=[B_rmask])
        DMA(sp, invcnt[:], c_invcnt, ds_c, writes=[B_invcnt])
        DMA(sp, tri[:], c_tri, ds_c, writes=[B_tri])
        DMA(sp, iota64[:], c_iota, ds_c, writes=[B_iota])
        DMA(sp, ecoff[:], c_ecoff, ds_c, writes=[B_ecoff])
        DMA(sp, lbl[:], lb_logits.rearrange("s (h p) -> p s h", p=128), ds_c, writes=[B_lbl], allow_slow_non_contiguous=True)
        DMA(sp, hgn[:], hg_norm_g.rearrange("(h p) -> p h", p=128), ds_c, writes=[B_hgn], allow_slow_non_contiguous=True)
        DMA(sp, pscale[:], pool_scale.rearrange("(h p) -> p h", p=128), ds_c, writes=[B_pscale], allow_slow_non_contiguous=True)
        dve.op(lambda e: e.memset(onesb[:], 1.0), [], [B_onesb])
        zrow = sbt(glob, "zrow", [128, D], BF16); B_zrow = Buf("zrow")
        dve.op(lambda e: e.memset(zrow[:], 0.0), [], [B_zrow])
        ds_zf = DSem(P, "ds_zfill")
        xg_v = xg_dram.ap().rearrange("(p r) d -> p r d", p=128)
        RPP = NROWS // 128
        ZCH = 16

        def zero_fill(j):
            sp.dma(lambda en, j=j: en.dma_start(out=xg_v[:, j * ZCH:(j + 1) * ZCH, :],
                                                in_=zrow[:].rearrange("p (o d) -> p o d", o=1).to_broadcast([128, ZCH, D])),
                   ds_zf, reads=[B_zrow], writes=[])
        NZF = RPP // ZCH
        TT(dve, lbv[:], lbl[:, 0, :], lbl[:, 1, :], ALU.subtract, [B_lbl], [B_lbv])
        A(lbv[:], lbv[:], AF.Sigmoid, [B_lbv], [B_lbv])
        TS(dve, omlv[:], lbv[:], -1.0, 1.0, ALU.mult, ALU.add, [B_lbv], [B_omlv])

        mix = ExitStack()
        with mix:
            w_in_sb = sbt(mix, "w_in_sb", [128, 8, 2560], BF16); B_win = [Buf(f"win{r}") for r in range(5)]
            wpool_sb = sbt(mix, "wpool_sb", [128, 4, 128], BF16); B_wpool = Buf("wpool")
            xb = [sbt(mix, f"xb{j}", [128, D], BF16) for j in range(4)]; B_xb = [Buf(f"xb{j}") for j in range(4)]
            ds_xb = [DSem(P, f"ds_xb{j}") for j in range(4)]
            xTb = sbt(mix, "xTb", [128, 8, BLK], BF16); B_xT = [Buf(f"xT{j}") for j in range(4)]
            vblk = sbt(mix, "vblk", [64, 8, 512], BF16); B_v = [Buf(f"v{c}") for c in range(8)]
            ubuf = sbt(mix, "ubuf", [128, 4, 528], F32); B_u = [Buf(f"u{g}") for g in range(4)]
            pp0 = sbt(mix, "pp0", [128, 528], F32); B_pp0 = Buf("pp0")
            pp1 = sbt(mix, "pp1", [128, 528], F32); B_pp1 = Buf("pp1")
            pfix = sbt(mix, "pfix", [128, 16], F32); B_pfix = Buf("pfix")
            pT = sbt(mix, "pT", [128, 4, 512], BF16); B_pT = [Buf(f"pT{g}") for g in range(4)]
            S_f = sbt(mix, "S_f", [128, 4, 128], F32); B_Sf = [Buf(f"Sf{h}") for h in range(4)]
            S_bf = sbt(mix, "S_bf", [128, 4, 8, 128], BF16); B_Sbf = [[Buf(f"Sbf{h}_{s}") for s in range(8)] for h in range(4)]
            NSET = 4
            names32 = ["sgf", "logf", "b", "eb", "sgT"]
            names16 = ["qt", "kt", "kdT"]
            T = [dict() for _ in range(NSET)]
            BT = [dict() for _ in range(NSET)]
            for s in range(NSET):
                for nm in names32:
                    T[s][nm] = sbt(mix, f"T{s}_{nm}", [128, 512], F32); BT[s][nm] = Buf(f"T{s}_{nm}")
                for nm in names16:
                    T[s][nm] = sbt(mix, f"T{s}_{nm}", [128, 512], BF16); BT[s][nm] = Buf(f"T{s}_{nm}")
                T[s]["kd"] = sbt(mix, f"T{s}_kd", [64, 8, 128], BF16); BT[s]["kd"] = Buf(f"T{s}_kd")
                T[s]["scm"] = sbt(mix, f"T{s}_scm", [64, 8, 64], BF16); BT[s]["scm"] = Buf(f"T{s}_scm")

            ds_win = [DSem(P, f"ds_win{r}") for r in range(5)]

            def load_x(n):
                for j in range(4):
                    r0 = n * BLK + j * 128
                    DMA(pool, xb[j][:], xloc[r0:r0 + 128, :], ds_xb[j], writes=[B_xb[j]])

            load_x(0)
            for r in (1, 2, 0, 3, 4):
                DMA(pool, w_in_sb[:, :, r * 512:(r + 1) * 512],
                    w_in[:, r * 512:(r + 1) * 512].rearrange("(c p) n -> p c n", p=128), ds_win[r], writes=[B_win[r]])
            ds_wp = DSem(P, "ds_wpool")
            DMA(pool, wpool_sb[:], w_pool.rearrange("g c d -> c g d"), ds_wp, writes=[B_wpool])
            dve.op(lambda e: e.memset(S_f[:], 0.0), [], B_Sf)
            dve.op(lambda e: e.memset(S_bf[:], 0.0), [], [b for hb in B_Sbf for b in hb])
            dve.op(lambda e: e.memset(ubuf[:], 0.0), [], B_u)

            TRB = 0
            IPB = [1, 2, 3]
            ipc = [0]

            def next_ip():
                i = IPB[ipc[0] % 3]
                ipc[0] += 1
                return i

            UB = [4, 5]
            SCB = 6
            OB = 7
            trv = pb[TRB][:].bitcast(BF16).rearrange("p (c t) -> p c t", t=128)
            scv = pb[SCB][:64, :].rearrange("p (c t) -> p c t", t=64)

            for n in range(NBLK):
                is_main = n >= 4
                mb = n - 4
                for j in range(4):
                    for c in range(8):
                        TR(trv[:, c, :], xb[j][:, c * 128:(c + 1) * 128], identb[:], [B_xb[j], B_identb], [BP[TRB]], inc=(c == 7))
                    xt_tok = CP(dve if j % 2 == 0 else act, xTb[:, :, j * 128:(j + 1) * 128], trv, [BP[TRB]], [B_xT[j]])
                if n + 1 < NBLK:
                    load_x(n + 1)
                if n >= 2:
                    sp.wait_tok(xt_tok)
                    for j in range((n - 2) * NZF // (NBLK - 2), (n - 1) * NZF // (NBLK - 2)):
                        zero_fill(j)
                for c in range(8):
                    bi = next_ip()
                    for kc in range(8):
                        MM(pb[bi][:64, :], xTb[:, kc, c * 64:(c + 1) * 64], w_in_sb[:, kc, 1024:1536], kc == 0, kc == 7,
                           [B_xT[c // 2], B_win[2]], [BP[bi]], inc=(kc == 7))
                    CP(act, vblk[:, c, :], pb[bi][:64, :], [BP[bi]], [B_v[c]])
                if is_main or n == 3:
                    for g in range(4):
                        bi = next_ip()
                        for kc in range(8):
                            MM(pb[bi][:], w_in_sb[:, kc, 2048 + g * 128:2048 + (g + 1) * 128], xTb[:, kc, :], kc == 0, kc == 7,
                               B_xT + [B_win[4]], [BP[bi]], inc=(kc == 7))
                        CP(dve, ubuf[:, g, 16:528], pb[bi][:], [BP[bi]], [B_u[g]])
                HS = range(4)
                fb, qb, gb = {}, {}, {}
                for h in HS:
                    bi = next_ip(); fb[h] = bi
                    for kc in range(8):
                        MM(pb[bi][:], w_in_sb[:, kc, 512 + h * 128:512 + (h + 1) * 128], xTb[:, kc, :], kc == 0, kc == 7,
                           B_xT + [B_win[1]], [BP[bi]], inc=(kc == 7))
                    A(T[h]["sgf"][:], pb[bi][:], AF.Sigmoid, [BP[bi]], [BT[h]["sgf"]])
                for h in HS:
                    t, bt = T[h], BT[h]
                    TS(dve, t["sgf"][:], t["sgf"][:], omlv[:, h:h + 1], lbv[:, h:h + 1], ALU.mult, ALU.add,
                       [bt["sgf"], B_omlv, B_lbv], [bt["sgf"]])
                for h in HS:
                    t, bt = T[h], BT[h]
                    A(t["logf"][:], t["sgf"][:], AF.Ln, [bt["sgf"]], [bt["logf"]])
                for h in HS:
                    t, bt = T[h], BT[h]
                    dve.op(lambda e, t=t: e.tensor_tensor_scan(out=t["b"][:], data0=rmask[:], data1=t["logf"][:], initial=0.0,
                                                                op0=ALU.mult, op1=ALU.add),
                           [bt["logf"], B_rmask], [bt["b"]])
                    TS(pool, t["sgf"][:], t["sgf"][:], -1.0, 1.0, ALU.mult, ALU.add, [bt["sgf"]], [bt["sgf"]])
                for h in HS:
                    t, bt = T[h], BT[h]
                    A(t["eb"][:], t["b"][:], AF.Exp, [bt["b"]], [bt["eb"]])
                    A(t["logf"][:], t["b"][:], AF.Exp, [bt["b"]], [bt["logf"]], scale=-1.0)
                for h in HS:
                    t, bt = T[h], BT[h]
                    TT(dve, t["kt"][:], t["sgf"][:], t["logf"][:], ALU.mult, [bt["sgf"], bt["logf"]], [bt["kt"]])
                    eb3 = t["eb"][:].rearrange("p (c j) -> p c j", j=64)
                    TT(dve, t["kdT"][:].rearrange("p (c j) -> p c j", j=64), t["kt"][:].rearrange("p (c j) -> p c j", j=64),
                       eb3[:, :, 63:64].to_broadcast([128, 8, 64]), ALU.mult, [bt["kt"], bt["eb"]], [bt["kdT"]])
                if is_main:
                    for h in HS:
                        bi = next_ip()
                        for kc in range(8):
                            MM(pb[bi][:], w_in_sb[:, kc, h * 128:(h + 1) * 128], xTb[:, kc, :], kc == 0, kc == 7,
                               B_xT + [B_win[0]], [BP[bi]], inc=(kc == 7))
                        A(T[h]["b"][:], pb[bi][:], AF.Silu, [BP[bi]], [BT[h]["b"]])
                    for h in HS:
                        bi = next_ip()
                        for kc in range(8):
                            MM(pb[bi][:], w_in_sb[:, kc, 1536 + h * 128:1536 + (h + 1) * 128], xTb[:, kc, :], kc == 0, kc == 7,
                               B_xT + [B_win[3]], [BP[bi]], inc=(kc == 7))
                        A(T[h]["sgT"][:], pb[bi][:], AF.Silu, [BP[bi]], [BT[h]["sgT"]])
                    for h in HS:
                        t, bt = T[h], BT[h]
                        TT(dve, t["qt"][:], t["b"][:], t["eb"][:], ALU.mult, [bt["b"], bt["eb"]], [bt["qt"]])
                for h in HS:
                    t, bt = T[h], BT[h]
                    for c in range(8):
                        TR(trv[:64, c, :], t["kdT"][:, c * 64:(c + 1) * 64], identb[:], [bt["kdT"], B_identb], [BP[TRB]], inc=(c == 7))
                    CP(act, t["kd"][:], trv[:64, :, :], [BP[TRB]], [bt["kd"]])
                if is_main:
                    for h in HS:
                        t, bt = T[h], BT[h]
                        for c in range(8):
                            MM(scv[:, c, :], t["kt"][:, c * 64:(c + 1) * 64], t["qt"][:, c * 64:(c + 1) * 64], True, True,
                               [bt["kt"], bt["qt"]], [BP[SCB]], inc=(c == 7))
                        TT(dve, t["scm"][:], scv, cmask[:].rearrange("p (o t) -> p o t", o=1).to_broadcast([64, 8, 64]), ALU.mult,
                           [BP[SCB], B_cmask], [bt["scm"]])
                OBK = [OB] + IPB

                def emit_U(c):
                    ub = UB[c % 2]
                    for h in HS:
                        MM(pb[ub][:, h * 128:(h + 1) * 128], T[h]["kd"][:, c, :], vblk[:, c, h * 128:(h + 1) * 128], True, True,
                           [BT[h]["kd"], B_v[c]], [BP[ub]], inc=(h == 3))

                emit_U(0)
                for c in range(8):
                    if c + 1 < 8:
                        emit_U(c + 1)
                    if is_main:
                        for h in HS:
                            t, bt = T[h], BT[h]
                            MM(pb[OBK[h]][:, c * 64:(c + 1) * 64], S_bf[:, h, c, :], t["qt"][:, c * 64:(c + 1) * 64], True, False,
                               [B_Sbf[h][c], bt["qt"]], [BP[OBK[h]]], inc=False)
                            MM(pb[OBK[h]][:, c * 64:(c + 1) * 64], vblk[:, c, h * 128:(h + 1) * 128], t["scm"][:, c, :], False, True,
                               [B_v[c], bt["scm"]], [BP[OBK[h]]], inc=True)
                    ub = UB[c % 2]
                    for h in HS:
                        t, bt = T[h], BT[h]
                        STT(S_f[:, h, :], S_f[:, h, :], t["eb"][:, c * 64 + 63:c * 64 + 64], pb[ub][:, h * 128:(h + 1) * 128],
                            ALU.mult, ALU.add, [B_Sf[h], bt["eb"], BP[ub]], [B_Sf[h]])
                    nslot = (c + 1) % 8
                    if is_main or (n == 3 and c == 7):
                        for h in HS:
                            CP(act, S_bf[:, h, nslot, :], S_f[:, h, :], [B_Sf[h]], [B_Sbf[h][nslot]])
                if is_main:
                    for h in HS:
                        A(T[h]["kdT"][:], pb[OBK[h]][:], AF.Square, [BP[OBK[h]]], [BT[h]["kdT"]])
                    for h in HS:
                        bs = UB[h % 2]
                        MM(pb[bs][:], onesb[:], T[h]["kdT"][:], True, True, [B_onesb, BT[h]["kdT"]], [BP[bs]], inc=True)
                        A(T[h]["logf"][:], pb[bs][:], AF.Ln, [BP[bs]], [BT[h]["logf"]], scale=1.0 / 128.0, bias=RMS_EPS)
                    for h in HS:
                        A(T[h]["logf"][:], T[h]["logf"][:], AF.Exp, [BT[h]["logf"]], [BT[h]["logf"]], scale=-0.5)
                    for h in HS:
                        t, bt = T[h], BT[h]
                        TT(dve, t["sgf"][:], pb[OBK[h]][:], t["logf"][:], ALU.mult, [BP[OBK[h]], bt["logf"]], [bt["sgf"]])
                        STT(mixT[:, h, mb * BLK:(mb + 1) * BLK], t["sgf"][:], hgn[:, h:h + 1], t["sgT"][:], ALU.mult, ALU.mult,
                            [bt["sgf"], B_hgn, bt["sgT"]], [B_mix[h][mb]])
                if is_main:
                    for g in range(4):
                        w = WINS[g]
                        src, Bsrc = ubuf[:, g, :], B_u[g]
                        d = 1
                        lvl = 0
                        while d < w:
                            dst, Bdst = (pp0, B_pp0) if lvl % 2 == 0 else (pp1, B_pp1)
                            lo = 2 * d - 1
                            TT(pool, dst[:, lo:528], src[:, lo:528], src[:, lo - d:528 - d], ALU.add, [Bsrc], [Bdst])
                            src, Bsrc = dst[:], Bdst
                            d *= 2
                            lvl += 1
                        STT(pT[:, g, :], src[:, 16:528], 1.0 / w, ubuf[:, g, 16:528], ALU.mult, ALU.subtract, [Bsrc, B_u[g]], [B_pT[g]])
                        if mb == 0:
                            TT(dve, pfix[:], src[:, 16:32], invcnt[:, g, :], ALU.mult, [Bsrc, B_invcnt], [B_pfix])
                            TT(dve, pT[:, g, 0:16], pfix[:], ubuf[:, g, 16:32], ALU.subtract, [B_pfix, B_u[g]], [B_pT[g]])
                        bi = next_ip()
                        MM(pb[bi][:], wpool_sb[:, g, :], pT[:, g, :], True, True, [B_wpool, B_pT[g]], [BP[bi]], inc=True)
                        A(mixT[:, 4 + g, mb * BLK:(mb + 1) * BLK], pb[bi][:], AF.Copy, [BP[bi], B_pscale], [B_mix[4 + g][mb]], scale=pscale[:, g:g + 1])
                if is_main or n == 3:
                    for g in range(4):
                        CP(pool, ubuf[:, g, 0:16], ubuf[:, g, 512:528], [B_u[g]], [B_u[g]])

            if debug == "mixer":
                ds_dbg = DSem(P, "ds_dbg")
                tk = DMA(sp, dbg["mixT"], mixT[:], ds_dbg, reads=[b for row in B_mix for b in row])
                sp.wait_tok(tk)
                P.emit()
                return nc
            P.barrier()
            P.emit()

        post = ExitStack()
        with post:
            h1 = sbt(post, "h1", [128, 16, D], F32); B_h1 = [[Buf(f"h1_{i}_{hf}") for hf in range(2)] for i in range(16)]
            h1T = sbt(post, "h1T", [128, 8, NMAIN], BF16); B_h1T = [Buf(f"h1T{i}") for i in range(16)]
            lng = sbt(post, "lng", [128, D], F32); B_lng = Buf("lng")
            lnb = sbt(post, "lnb", [128, D], F32); B_lnb = Buf("lnb")
            ds_ln = DSem(P, "ds_ln")
            DMA(sp, lng[:], ln1_g.partition_broadcast(128), ds_ln, writes=[B_lng])
            DMA(sp, lnb[:], ln1_b.partition_broadcast(128), ds_ln, writes=[B_lnb])
            stats = sbt(post, "stats", [128, 2, 6], F32); B_stats = Buf("stats")
            mv = sbt(post, "mv", [128, 2], F32); B_mv = Buf("mv")
            rstd = sbt(post, "rstd", [128, 1], F32); B_rstd = Buf("rstd")

            nmr = sbt(post, "nmr", [128, 1], F32); B_nmr = Buf("nmr")

            def layer_norm(src, Bsrc_list, dst, Bdst_list, eng_b=pool, on_act=False):
                for hf in range(2):
                    dve.op(lambda e, hf=hf: e.bn_stats(out=stats[:, hf, :], in_=src[:, hf * 512:(hf + 1) * 512]), Bsrc_list, [B_stats])
                dve.op(lambda e: e.bn_aggr(out=mv[:], in_=stats[:].rearrange("p a b -> p (a b)")), [B_stats], [B_mv])
                A(rstd[:], mv[:, 1:2], AF.Ln, [B_mv], [B_rstd], bias=LN_EPS)
                A(rstd[:], rstd[:], AF.Exp, [B_rstd], [B_rstd], scale=-0.5)
                if on_act:
                    TS(dve, nmr[:], mv[:, 0:1], rstd[:, 0:1], -1.0, ALU.mult, ALU.mult, [B_mv, B_rstd], [B_nmr])
                    A(dst, src, AF.Identity, Bsrc_list + [B_rstd, B_nmr], Bdst_list, scale=rstd[:, 0:1], bias=nmr[:, 0:1])
                else:
                    TS(dve, dst, src, mv[:, 0:1], rstd[:, 0:1], ALU.subtract, ALU.mult, Bsrc_list + [B_mv, B_rstd], Bdst_list)
                TT(dve, dst, dst, lng[:], ALU.mult, Bdst_list + [B_lng], Bdst_list)
                TT(eng_b, dst, dst, lnb[:], ALU.add, Bdst_list + [B_lnb], Bdst_list)

            p4 = ExitStack()
            with p4:
                wout_sb = sbt(p4, "wout_sb", [128, 8, D], BF16); B_wout = Buf("wout")
                wr_sb = sbt(p4, "wr_sb", [128, 8, 64], F32); B_wr = Buf("wr")
                rbias = sbt(p4, "rbias", [128, 64], F32); B_rbias = Buf("rbias")
                xres = [sbt(p4, f"xres{i}", [128, D], F32) for i in range(2)]; B_xres = [Buf(f"xres{i}") for i in range(2)]
                ds_xres = [DSem(P, f"ds_xres{i}") for i in range(2)]
                rres = sbt(p4, "rres", [128, D], F32); B_rres = Buf("rres")
                hhi2 = [sbt(p4, f"hhi{q}", [128, D], BF16) for q in range(2)]; B_hhi2 = [Buf(f"hhi{q}") for q in range(2)]
                maskb = sbt(p4, "maskb", [128, 16, 64], BF16); B_mask = [Buf(f"mask{i}") for i in range(16)]
                hlo = sbt(p4, "hlo", [128, D], BF16); B_hlo = Buf("hlo")
                loT = sbt(p4, "loT", [128, 8, 128], BF16); B_loT = Buf("loT")
                whi = sbt(p4, "whi", [128, 8, 64], BF16); B_whi = Buf("whi")
                wlo = sbt(p4, "wlo", [128, 8, 64], BF16); B_wlo = Buf("wlo")
                sc_all = sbt(p4, "sc_all", [128, 16, 64], F32); B_sc = [Buf(f"sc{i}") for i in range(16)]
                cumb = sbt(p4, "cumb", [128, 17, 64], BF16); B_cum = [Buf(f"cum{i}") for i in range(17)]
                hhiB = [sbt(p4, f"hhiB{q}", [128, D], BF16) for q in range(2)]; B_hhiB = [Buf(f"hhiB{q}") for q in range(2)]
                G = 4
                rt = {}
                Brt = {}
                for nm, shp in [("sel", [128, G * 64]), ("eq", [128, G * 64]), ("sel2", [128, G * 64]), ("selm", [128, G * 64]), ("w", [128, G * 64]),
                                ("dall", [128, G * 64]),
                                ("m1", [128, G * 8]), ("m2", [128, G * 8]), ("gs", [128, G * 8]), ("g8", [128, G * 8]), ("pen", [128, G * 8]),
                                ("t8", [128, G * 8]), ("i8f", [128, G * 8]), ("d8f", [128, G * 8]), ("okm", [128, G * 8]),
                                ("wsum", [128, G]), ("rs", [128, G])]:
                    rt[nm] = sbt(p4, f"rt_{nm}", shp, F32); Brt[nm] = Buf(f"rt_{nm}")
                for new_nm, old_nm in (("oh", "eq"), ("junk", "sel2"), ("ovf", "selm")):
                    rt[new_nm], Brt[new_nm] = rt[old_nm], Brt[old_nm]
                rt_i8 = sbt(p4, "rt_i8", [128, G * 8], U32); B_i8 = Buf("rt_i8")
                ds_w4 = DSem(P, "ds_w4")
                DMA(pool, wout_sb[:], w_out.rearrange("(c p) n -> p c n", p=128), ds_w4, writes=[B_wout])
                DMA(sp, wr_sb[:], w_router.rearrange("(c p) n -> p c n", p=128), ds_ln, writes=[B_wr])
                DMA(sp, rbias[:], router_bias.partition_broadcast(128), ds_ln, writes=[B_rbias])
                CP(dve, whi[:], wr_sb[:], [B_wr], [B_whi])
                TT(dve, wlo[:], wr_sb[:], whi[:], ALU.subtract, [B_wr, B_whi], [B_wlo])
                dve.op(lambda e: e.memset(cumb[:, 0, :], 0.0), [], [B_cum[0]])

                ds_push = [DSem(P, f"ds_push{k}") for k in range(16)]

                def load_xres(i):
                    DMA(sp, xres[i % 2][:], xloc[NMAIN + i * 128:NMAIN + (i + 1) * 128, :], ds_xres[i % 2], writes=[B_xres[i % 2]])

                def bc3(ap2, n_mid, n_in):
                    return ap2.rearrange("p (g o) -> p g o", o=1).to_broadcast([128, n_mid, n_in])

                def stage_a1(i):
                    blk = i // 4
                    for hf in range(2):
                        for j in range(8):
                            MM(pb[hf][:], mixT[:, j, i * 128:(i + 1) * 128], wout_sb[:, j, hf * 512:(hf + 1) * 512], j == 0, j == 7,
                               [B_mix[j][blk], B_wout], [BP[hf]], inc=(j == 7))
                        STT(rres[:, hf * 512:(hf + 1) * 512], xres[i % 2][:, hf * 512:(hf + 1) * 512], ALPHA, pb[hf][:], ALU.mult, ALU.add,
                            [B_xres[i % 2], BP[hf]], [B_rres])
                    layer_norm(rres[:], [B_rres], h1[:, i, :], B_h1[i], eng_b=dve)

                def stage_a2(i):
                    hhi, B_hhi = hhi2[i % 2], B_hhi2[i % 2]
                    CP(act, hhi[:], h1[:, i, :], B_h1[i], [B_hhi])
                    TT(dve, hlo[:], h1[:, i, :], hhi[:], ALU.subtract, B_h1[i] + [B_hhi], [B_hlo])
                    tv2 = pb[2][:].bitcast(BF16).rearrange("p (c t) -> p c t", t=128)
                    tv3 = pb[3][:].bitcast(BF16).rearrange("p (c t) -> p c t", t=128)
                    for c in range(8):
                        TR(tv2[:, c, :], hhi[:, c * 128:(c + 1) * 128], identb[:], [B_hhi, B_identb], [BP[2]], inc=(c == 7))
                    for c in range(8):
                        TR(tv3[:, c, :], hlo[:, c * 128:(c + 1) * 128], identb[:], [B_hlo, B_identb], [BP[3]], inc=(c == 7))
                    CP(act, h1T[:, :, i * 128:(i + 1) * 128], tv2, [BP[2]], [B_h1T[i]])
                    CP(dve, loT[:], tv3, [BP[3]], [B_loT])
                    nmm = 0
                    for (lt, Bl, rw, Br) in ((h1T, B_h1T[i], whi, B_whi), (None, B_loT, whi, B_whi), (h1T, B_h1T[i], wlo, B_wlo)):
                        for c in range(8):
                            lhs = loT[:, c, :] if lt is None else h1T[:, c, i * 128:(i + 1) * 128]
                            MM(pb[4][:, 0:64], lhs, rw[:, c, :], nmm == 0, nmm == 23, [Bl, Br], [BP[4]], inc=(nmm == 23))
                            nmm += 1
                    A(sc_all[:, i, :], pb[4][:, 0:64], AF.Exp, [BP[4]], [B_sc[i]], scale=-1.0)
                    TS(dve, sc_all[:, i, :], sc_all[:, i, :], 1.0, None, ALU.add, ALU.bypass, [B_sc[i]], [B_sc[i]])
                    dve.op(lambda e, i=i: e.reciprocal(out=sc_all[:, i, :], in_=sc_all[:, i, :]), [B_sc[i]], [B_sc[i]])

                def stage_b(g):
                    tl = list(range(G * g, G * g + G))
                    Bsel = [Brt["sel"]]
                    Bsc = [B_sc[i] for i in tl]
                    Bgw = [B_gw[i] for i in tl]
                    sc2d = sc_all[:, G * g:G * g + G, :].rearrange("p t e -> p (t e)")
                    sel2d = rt["sel"][:]
                    sel44 = sel2d.rearrange("p (q e) -> p q e", e=8)
                    TT(dve, sel2d.rearrange("p (t e) -> p t e", e=64), sc_all[:, G * g:G * g + G, :],
                       rbias[:].rearrange("p (o e) -> p o e", o=1).to_broadcast([128, G, 64]), ALU.add, Bsc + [B_rbias], Bsel)
                    gw2d = gw_all[:, G * g:G * g + G, :].rearrange("p t e -> p (t e)")
                    NQ = G * 8
                    dve.op(lambda e: e.tensor_reduce(out=rt["m1"][:], in_=sel44, axis=AX.X, op=ALU.max), Bsel, [Brt["m1"]])
                    TT(dve, rt["eq"][:].rearrange("p (q e) -> p q e", e=8), sel44, bc3(rt["m1"][:], NQ, 8), ALU.is_equal, Bsel + [Brt["m1"]], [Brt["eq"]])
                    STT(rt["sel2"][:], rt["eq"][:], -1.0e9, sel2d, ALU.mult, ALU.add, [Brt["eq"]] + Bsel, [Brt["sel2"]])
                    dve.op(lambda e: e.tensor_reduce(out=rt["m2"][:], in_=rt["sel2"][:].rearrange("p (q e) -> p q e", e=8), axis=AX.X, op=ALU.max),
                           [Brt["sel2"]], [Brt["m2"]])
                    TT(dve, rt["gs"][:], rt["m1"][:], rt["m2"][:], ALU.add, [Brt["m1"], Brt["m2"]], [Brt["gs"]])
                    for t in range(G):
                        dve.op(lambda e, t=t: e.max(out=rt["g8"][:, t * 8:(t + 1) * 8], in_=rt["gs"][:, t * 8:(t + 1) * 8]), [Brt["gs"]], [Brt["g8"]])
                    g83 = rt["g8"][:].rearrange("p (t k) -> p t k", k=8)
                    TT(dve, rt["pen"][:].rearrange("p (t k) -> p t k", k=8), rt["gs"][:].rearrange("p (t k) -> p t k", k=8),
                       g83[:, :, 3:4].to_broadcast([128, G, 8]), ALU.is_lt, [Brt["gs"], Brt["g8"]], [Brt["pen"]])
                    TS(dve, rt["pen"][:], rt["pen"][:], -1.0e9, None, ALU.mult, ALU.bypass, [Brt["pen"]], [Brt["pen"]])
                    TT(dve, rt["selm"][:].rearrange("p (q e) -> p q e", e=8), sel44, bc3(rt["pen"][:], NQ, 8), ALU.add, Bsel + [Brt["pen"]], [Brt["selm"]])
                    for t in range(G):
                        dve.op(lambda e, t=t: e.max(out=rt["t8"][:, t * 8:(t + 1) * 8], in_=rt["selm"][:, t * 64:(t + 1) * 64]), [Brt["selm"]], [Brt["t8"]])
                    t83 = rt["t8"][:].rearrange("p (t k) -> p t k", k=8)
                    selm3 = rt["selm"][:].rearrange("p (t e) -> p t e", e=64)
                    w3 = rt["w"][:].rearrange("p (t e) -> p t e", e=64)
                    TT(dve, w3, selm3, t83[:, :, 7:8].to_broadcast([128, G, 64]), ALU.is_ge, [Brt["selm"], Brt["t8"]], [Brt["w"]])
                    TT(dve, rt["w"][:], rt["w"][:], sc2d, ALU.mult, [Brt["w"]] + Bsc, [Brt["w"]])
                    dve.op(lambda e: e.tensor_reduce(out=rt["wsum"][:], in_=w3, axis=AX.X, op=ALU.add), [Brt["w"]], [Brt["wsum"]])
                    dve.op(lambda e: e.reciprocal(out=rt["rs"][:], in_=rt["wsum"][:]), [Brt["wsum"]], [Brt["rs"]])
                    STT(gw_all[:, G * g:G * g + G, :], w3, 2.5, bc3(rt["rs"][:], G, 64), ALU.mult, ALU.mult, [Brt["w"], Brt["rs"]], Bgw)
                    if debug is not None:
                        return
                    yield
                    for t, i in enumerate(tl):
                        TS(dve, maskb[:, i, :], gw_all[:, i, :], 0.0, None, ALU.is_gt, ALU.bypass, [B_gw[i]], [B_mask[i]])
                        TT(dve, cumb[:, i + 1, :], cumb[:, i, :], maskb[:, i, :], ALU.add, [B_cum[i], B_mask[i]], [B_cum[i + 1]])
                        MM(pb[5][:, t * 64:(t + 1) * 64], tri[:], maskb[:, i, :], True, False, [B_tri, B_mask[i]], [BP[5]], inc=False)
                        MM(pb[5][:, t * 64:(t + 1) * 64], onesb[:], cumb[:, i, :], False, True, [B_onesb, B_cum[i]], [BP[5]], inc=(t == G - 1))
                    ec3 = ecoff[:].rearrange("p (o e) -> p o e", o=1).to_broadcast([128, G, 64])
                    io3 = iota64[:].rearrange("p (o e) -> p o e", o=1).to_broadcast([128, G, 64])
                    pos3 = pb[5][:, 0:G * 64].rearrange("p (t e) -> p t e", e=64)
                    dall3 = rt["dall"][:].rearrange("p (t e) -> p t e", e=64)
                    TT(dve, dall3, pos3, ec3, ALU.add, [BP[5], B_ecoff], [Brt["dall"]])
                    TS(dve, rt["ovf"][:], pb[5][:, 0:G * 64], float(CAP), 1.0e6, ALU.is_ge, ALU.mult, [BP[5]], [Brt["ovf"]])
                    TT(dve, rt["dall"][:], rt["dall"][:], rt["ovf"][:], ALU.add, [Brt["dall"], Brt["ovf"]], [Brt["dall"]])
                    yield
                    for t, i in enumerate(tl):
                        dve.op(lambda e, i=i: e.max(out=w8_all[:, i, :], in_=gw_all[:, i, :]), [B_gw[i]], [B_w8[i]])
                        dve.op(lambda e, i=i, t=t: e.max_index(out=rt_i8[:, t * 8:(t + 1) * 8], in_max=w8_all[:, i, :], in_values=gw_all[:, i, :]),
                               [B_gw[i], B_w8[i]], [B_i8])
                    CP(dve, rt["i8f"][:], rt_i8[:], [B_i8], [Brt["i8f"]])
                    i83 = rt["i8f"][:].rearrange("p (t k) -> p t k", k=8)
                    d83 = rt["d8f"][:].rearrange("p (t k) -> p t k", k=8)
                    oh3 = rt["oh"][:].rearrange("p (t e) -> p t e", e=64)
                    jk3 = rt["junk"][:].rearrange("p (t e) -> p t e", e=64)
                    for k in range(8):
                        TT(dve, oh3, io3, i83[:, :, k:k + 1].to_broadcast([128, G, 64]), ALU.is_equal, [B_iota, Brt["i8f"]], [Brt["oh"]])
                        TT(dve, rt["junk"][:], rt["oh"][:], rt["dall"][:], ALU.mult, [Brt["oh"], Brt["dall"]], [Brt["junk"]])
                        dve.op(lambda e, k=k: e.tensor_reduce(out=d83[:, :, k], in_=jk3, axis=AX.X, op=ALU.add), [Brt["junk"]], [Brt["d8f"]])
                    CP(dve, dest8[:, G * g * 8:(G * g + G) * 8], rt["d8f"][:], [Brt["d8f"]], [B_d8[i] for i in tl])
                    TS(dve, rt["okm"][:], rt["d8f"][:], float(NROWS), None, ALU.is_lt, ALU.bypass, [Brt["d8f"]], [Brt["okm"]])
                    w8g = w8_all[:, G * g:G * g + G, :].rearrange("p t k -> p (t k)")
                    TT(dve, w8g, w8g, rt["okm"][:], ALU.mult, [B_w8[i] for i in tl] + [Brt["okm"]], [B_w8[i] for i in tl])
                    yield
                    for t, i in enumerate(tl):
                        hb_, Bhb = hhiB[i % 2], B_hhiB[i % 2]
                        CP(act, hb_[:].rearrange("t (c p) -> t c p", p=128), h1[:, i, :].rearrange("t (p c) -> t c p", c=8), B_h1[i], [Bhb])
                        for k in range(8):
                            pool.dma(lambda e, i=i, k=k, hb_=hb_: e.indirect_dma_start(
                                out=xg_dram[:, :], out_offset=bass.IndirectOffsetOnAxis(ap=dest8[:, i * 8 + k:i * 8 + k + 1], axis=0), in_=hb_[:], in_offset=None,
                                bounds_check=P.bc_reg(e, NROWS - 1), oob_is_err=False), ds_push[(i % 2) * 8 + k], reads=[Bhb, B_d8[i]], writes=[])

                def drain(gen):
                    for _ in gen:
                        pass

                load_xres(0)
                stage_a1(0)
                pending = None
                for i in range(16):
                    if i + 1 < 16:
                        load_xres(i + 1)
                        stage_a1(i + 1)
                    if debug == "p4a":
                        continue
                    stage_a2(i)
                    if pending is not None:
                        if next(pending, "done") == "done":
                            pending = None
                    if i % G == G - 1:
                        if pending is not None:
                            drain(pending)
                        pending = stage_b(i // G)
                if pending is not None:
                    drain(pending)

                if debug in ("p4", "p4a", "p4b"):
                    ds_dbg = DSem(P, "ds_dbg")
                    tk = DMA(sp, dbg["h1"].rearrange("(i p) d -> p i d", p=128), h1[:], ds_dbg, reads=[b for r in B_h1 for b in r])
                    if debug != "p4a":
                        tk = DMA(sp, dbg["gw"].rearrange("(i p) e -> p i e", p=128), gw_all[:], ds_dbg, reads=B_gw)
                    sp.wait_tok(tk)
                    P.emit()
                    return nc
                P.barrier()
                P.emit()

            moe = ExitStack()
            with moe:
                NWB = 2
                wg = [sbt(moe, f"wg{i}", [128, 8, 256], BF16) for i in range(NWB)]
                wu = [sbt(moe, f"wu{i}", [128, 8, 256], BF16) for i in range(NWB)]
                wd = [sbt(moe, f"wd{i}", [128, 2, D], BF16) for i in range(NWB)]
                B_wg = [Buf(f"wg{i}") for i in range(NWB)]; B_wu = [Buf(f"wu{i}") for i in range(NWB)]; B_wd = [Buf(f"wd{i}") for i in range(NWB)]
                xs = [sbt(moe, f"xs{i}", [128, D], BF16) for i in range(4)]; B_xs = [Buf(f"xs{i}") for i in range(4)]
                flat = mixT[:].rearrange("p a b -> p (a b)")
                xgT = [flat[:, i * 8 * CAP:(i + 1) * 8 * CAP].rearrange("p (c t) -> p c t", t=CAP) for i in range(2)]
                B_xgT = [[Buf(f"xgT{i}_{q}") for q in range(CAP // 128)] for i in range(2)]
                sl = [sbt(moe, f"sl{i}", [128, 512], BF16) for i in range(2)]; B_sl = [Buf(f"sl{i}") for i in range(2)]
                hT = [sbt(moe, f"hT{i}", [128, 2, 512], BF16) for i in range(2)]; B_hT = [[Buf(f"hT{i}_{hc}") for hc in range(2)] for i in range(2)]
                ysb = [flat[:, 12288 + i * D:12288 + (i + 1) * D] for i in range(3)]; B_ysb = [Buf(f"ysb{i}") for i in range(3)]
                NYK = 8
                yk = [flat[:, 8192 + i * D:8192 + (i + 1) * D] for i in range(4)] + [sbt(moe, f"yk{i}", [128, D], BF16) for i in range(4, NYK)]
                B_yk = [Buf(f"yk{i}") for i in range(NYK)]
                ostg = [sbt(moe, f"ostg{i}", [128, D], F32) for i in range(2)]; B_ostg = [Buf(f"ostg{i}") for i in range(2)]
                ds_out = [DSem(P, f"ds_out{i}") for i in range(2)]
                ds_yst = [DSem(P, f"ds_yst{i}") for i in range(3)]
                ds_pull = [DSem(P, f"ds_pull{i}") for i in range(8)]
                DMA(sp, lng[:], ln2_g.partition_broadcast(128), None, writes=[B_lng])
                DMA(sp, lnb[:], ln2_b.partition_broadcast(128), None, writes=[B_lnb])
                for q in range(NYK):
                    pool.op(lambda e, q=q: e.memset(yk[q][:], 0.0), [], [B_yk[q]])

                def load_expert(e):
                    s_ = e % NWB
                    if e < NEXP:
                        g_ap, u_ap, d_ap = w_gate[e], w_up[e], w_down[e]
                    else:
                        g_ap, u_ap, d_ap = ws_gate, ws_up, ws_down
                    lay = "(p c) n -> p c n" if e < NEXP else "(c p) n -> p c n"
                    DMA(pool, wg[s_][:], g_ap.rearrange(lay, p=128), None, writes=[B_wg[s_]])
                    DMA(pool, wu[s_][:], u_ap.rearrange(lay, p=128), None, writes=[B_wu[s_]])
                    DMA(pool, wd[s_][:], d_ap.rearrange("(c p) n -> p c n", p=128), None, writes=[B_wd[s_]])

                experts = list(range(n_exp)) + [NEXP]
                load_expert(experts[0])
                for i in range(16):
                    for hf in range(2):
                        A(h1[:, i, hf * 512:(hf + 1) * 512], h1[:, i, hf * 512:(hf + 1) * 512], AF.Copy, [B_h1[i][hf]], [B_h1[i][hf]], scale=ALPHA)
                TRBS = [0, 1]
                trvs = [pb[b_][:].bitcast(BF16).rearrange("p (c t) -> p c t", t=128) for b_ in TRBS]
                GUB = [2, 3, 4]
                guc = [0]
                YB = [5, 6, 7]
                yc = 0
                uidx = 0
                xsc = 0
                ysc = 0
                evc = 0
                NSB = CAP // 128

                def prep_loads(ei, e):
                    for q in range(NSB):
                        DMA(sp, xs[q][:], xg_dram[e * CAP + q * 128:e * CAP + (q + 1) * 128, :], None, writes=[B_xs[q]])

                def prep_transposes(ei, e):
                    xb_ = ei % 2
                    for q in range(NSB):
                        tb = TRBS[q % 2]
                        for c in range(8):
                            TR(trvs[q % 2][:, c, :], xs[q][:, c * 128:(c + 1) * 128], identb[:], [B_xs[q], B_identb], [BP[tb]], inc=(c == 7))
                        CP(act if q % 2 == 0 else dve, xgT[xb_][:, :, q * 128:(q + 1) * 128], trvs[q % 2], [BP[tb]], [B_xgT[xb_][q]])

                if experts[0] < NEXP:
                    prep_loads(0, experts[0])
                    prep_transposes(0, experts[0])
                for ei, e in enumerate(experts):
                    s_ = e % NWB
                    xb_ = ei % 2
                    nxt = experts[ei + 1] if ei + 1 < len(experts) else None
                    if nxt is not None:
                        load_expert(nxt)
                        if nxt < NEXP:
                            prep_loads(ei + 1, nxt)
                    nblk = 1 if e < NEXP else 4
                    W = CAP if e < NEXP else 512
                    for blk in range(nblk):
                        hb = uidx % 2
                        uidx += 1
                        for hc in range(2):
                            gu = []
                            for (wt, Bw) in ((wg, B_wg), (wu, B_wu)):
                                bk = GUB[guc[0] % 3]
                                guc[0] += 1
                                gu.append(bk)
                                for kc in range(8):
                                    if e < NEXP:
                                        rhs, Br = xgT[xb_][:, kc, :], B_xgT[xb_]
                                    else:
                                        rhs, Br = h1T[:, kc, blk * 512:(blk + 1) * 512], B_h1T[blk * 4:(blk + 1) * 4]
                                    MM(pb[bk][:, 0:W], wt[s_][:, kc, hc * 128:(hc + 1) * 128], rhs, kc == 0, kc == 7,
                                       [Bw[s_]] + Br, [BP[bk]], inc=(kc == 7))
                            A(sl[hc][:, 0:W], pb[gu[0]][:, 0:W], AF.Silu, [BP[gu[0]]], [B_sl[hc]])
                            TT(dve, hT[hb][:, hc, 0:W], sl[hc][:, 0:W], pb[gu[1]][:, 0:W], ALU.mult, [B_sl[hc], BP[gu[1]]], [B_hT[hb][hc]])
                        if blk == 0 and nxt is not None and nxt < NEXP:
                            prep_transposes(ei + 1, nxt)
                        for tl in range(W // 128):
                            if e < NEXP:
                                yi = ysc % 3
                                ysc += 1
                            for hf in range(2):
                                yb = YB[yc % 3]
                                yc += 1
                                for hc in range(2):
                                    MM(pb[yb][:], hT[hb][:, hc, tl * 128:(tl + 1) * 128], wd[s_][:, hc, hf * 512:(hf + 1) * 512], hc == 0, hc == 1,
                                       [B_hT[hb][hc], B_wd[s_]], [BP[yb]], inc=(hc == 1))
                                if e < NEXP:
                                    CP(act if evc % 2 == 0 else dve, ysb[yi][:, hf * 512:(hf + 1) * 512], pb[yb][:], [BP[yb]], [B_ysb[yi]])
                                    evc += 1
                                else:
                                    i = blk * 4 + tl
                                    acc = h1[:, i, hf * 512:(hf + 1) * 512]
                                    TT(dve, acc, pb[yb][:], acc, ALU.add, [BP[yb], B_h1[i][hf]], [B_h1[i][hf]])
                            if e < NEXP:
                                act.dma(lambda en, e=e, tl=tl, yi=yi: en.dma_start(out=y_dram[e * CAP + tl * 128:e * CAP + (tl + 1) * 128, :], in_=ysb[yi][:]),
                                        ds_yst[yi], reads=[B_ysb[yi]], writes=[])
                P.barrier()
                w8f = w8_all[:].rearrange("p t k -> p (t k)")
                w8hi = sl[0][:, 0:128]; B_w8hi = Buf("w8hi")
                w8lo = hT[0][:, 0, 0:128]; B_w8lo = Buf("w8lo")
                w8lf = ostg[0][:, 0:128]; B_w8lf = Buf("w8lf")
                CP(dve, w8hi, w8f, B_w8, [B_w8hi])
                TT(dve, w8lf, w8f, w8hi, ALU.subtract, B_w8 + [B_w8hi], [B_w8lf])
                CP(dve, w8lo, w8lf, [B_w8lf], [B_w8lo])
                Dm = [[wt[b_][:].rearrange("p c n -> p (c n)")[:, 0:1024].rearrange("p (k m) -> p k m", m=128) for wt in (wg, wu)] for b_ in range(2)]
                B_Dm = [[Buf(f"Dm{b_}_{j}") for j in range(2)] for b_ in range(2)]
                id3 = identb[:].rearrange("p (o m) -> p o m", o=1).to_broadcast([128, 8, 128])
                gk = 0
                def make_D(i):
                    bf_ = i % 2
                    for j, (wsrc, Bsrc) in enumerate(((w8hi, B_w8hi), (w8lo, B_w8lo))):
                        TT(dve, Dm[bf_][j], id3, wsrc[:, i * 8:(i + 1) * 8].rearrange("p (k o) -> p k o", o=1).to_broadcast([128, 8, 128]), ALU.mult,
                           [B_identb, Bsrc], [B_Dm[bf_][j]])

                make_D(0)
                for i in range(16):
                    bf_ = i % 2
                    for k in range(8):
                        r = gk % NYK
                        gk += 1
                        pool.dma(lambda en, i=i, k=k, r=r: en.indirect_dma_start(
                            out=yk[r][:], out_offset=None, in_=y_dram[:, :], in_offset=bass.IndirectOffsetOnAxis(ap=dest8[:, i * 8 + k:i * 8 + k + 1], axis=0),
                            bounds_check=P.bc_reg(en, NROWS - 1), oob_is_err=False), ds_pull[r], reads=[B_d8[i]], writes=[B_yk[r]])
                        for hf in range(2):
                            bk = 2 * bf_ + hf
                            MM(pb[bk][:], Dm[bf_][0][:, k, :], yk[r][:, hf * 512:(hf + 1) * 512], k == 0, False, [B_Dm[bf_][0], B_yk[r]], [BP[bk]], inc=False)
                            MM(pb[bk][:], Dm[bf_][1][:, k, :], yk[r][:, hf * 512:(hf + 1) * 512], False, k == 7, [B_Dm[bf_][1], B_yk[r]], [BP[bk]], inc=True)
                    if i + 1 < 16:
                        make_D(i + 1)
                    for hf in range(2):
                        acc = h1[:, i, hf * 512:(hf + 1) * 512]
                        TT(dve, acc, pb[2 * bf_ + hf][:], acc, ALU.add, [BP[2 * bf_ + hf], B_h1[i][hf]], [B_h1[i][hf]])
                    o = ostg[i % 2]
                    layer_norm(h1[:, i, :], B_h1[i], o[:], [B_ostg[i % 2]], eng_b=dve, on_act=True)
                    sp.dma(lambda en, i=i, o=o: en.dma_start(out=out[i * 128:(i + 1) * 128, :], in_=o[:]), ds_out[i % 2], reads=[B_ostg[i % 2]], writes=[])
                for d in ds_out:
                    sp.wait_tok(Tok(d.sem, d.count, None))
                P.emit()
    return nc


_CACHE = {}


def _consts(hf):
    identb = np.eye(128, dtype=np.float32).astype(ml_dtypes.bfloat16)
    identf = np.eye(128, dtype=np.float32)
    s = np.arange(64)[:, None]
    t = np.arange(64)[None, :]
    cmask = (t >= s).astype(np.float32)
    rmask = np.ones((128, 512), np.float32)
    rmask[:, ::64] = 0.0
    inv = np.zeros((128, 4, 16), np.float32)
    for g, w in enumerate(WINS):
        if hf == 0:
            inv[:, g, :] = 1.0 / np.minimum(np.arange(1, 17), w).astype(np.float32)
        else:
            inv[:, g, :] = 1.0 / w
    tp = np.arange(128)[:, None]
    tt = np.arange(128)[None, :]
    tri = (tp < tt).astype(np.float32).astype(ml_dtypes.bfloat16)
    iota = np.tile(np.arange(64, dtype=np.float32), (128, 1))
    ecoff = iota * float(CAP)
    return dict(c_identb=identb, c_identf=identf, c_cmask=cmask, c_rmask=rmask, c_invcnt=inv, c_tri=tri, c_iota=iota, c_ecoff=ecoff)


def make_in_maps(inputs, n_cores=8):
    x = np.asarray(inputs["x"], np.float32)
    sq = lambda k: np.ascontiguousarray(np.asarray(inputs[k], np.float32)[0])
    shared = {k: sq(k) for k in ["w_in", "hg_norm_g", "w_pool", "pool_scale", "w_out", "ln1_g", "ln1_b", "w_router", "router_bias",
                                 "w_gate", "w_up", "w_down", "ws_gate", "ws_up", "ws_down", "ln2_g", "ln2_b"]}
    shared["hg_lb_logits"] = np.ascontiguousarray(np.asarray(inputs["hg_lb_logits"], np.float32))
    in_maps = []
    for c in range(n_cores):
        b, hf = c // 2, c % 2
        xl = np.zeros((NLOC, D), np.float32)
        if hf == 1:
            xl[:NMAIN] = x[b, :NMAIN]
        xl[NMAIN:] = x[b, hf * NMAIN:(hf + 1) * NMAIN]
        m = dict(shared)
        m["xloc"] = xl
        m.update(_consts(hf))
        in_maps.append(m)
    return in_maps


def kernel(**inputs):
    if "nc" not in _CACHE:
        _CACHE["nc"] = build()
    nc = _CACHE["nc"]
    in_maps = make_in_maps(inputs)
    res = run_bass_kernel_spmd(nc, in_maps, core_ids=list(range(8)))
    x = inputs["x"]
    outp = np.zeros(x.shape, np.float32)
    for c in range(8):
        b, hf = c // 2, c % 2
        outp[b, hf * NMAIN:(hf + 1) * NMAIN] = res.results[c]["out"]
    return outp
```

```python
import numpy as np
import ml_dtypes
from contextlib import ExitStack
import concourse.bass as bass
import concourse.mybir as mybir
from concourse.bass_utils import run_bass_kernel_spmd

F32 = mybir.dt.float32
BF16 = mybir.dt.bfloat16
AF = mybir.ActivationFunctionType
ALU = mybir.AluOpType
AX = mybir.AxisListType

SEM_ROLL = 6000


class Tok:
    __slots__ = ("sem", "val", "eng")

    def __init__(self, sem, val, eng):
        self.sem, self.val, self.eng = sem, val, eng


class Buf:
    __slots__ = ("name", "w", "r")

    def __init__(self, name):
        self.name, self.w, self.r = name, None, []


class DSem:
    def __init__(self, prog, name):
        self.sem = prog.nc.alloc_semaphore(name)
        self.count = 0
        prog.dsems.append(self)


class Eng:
    def __init__(self, prog, name, is_pe=False, strict=False):
        self.prog, self.name, self.is_pe, self.strict = prog, name, is_pe, strict
        self.items = []
        self.waited = {}
        self.nsem = 0
        self.sem = None
        self.count = 0
        self.pend_r, self.pend_w = [], []
        self._roll()

    def _roll(self):
        self.sem = self.prog.nc.alloc_semaphore(f"c_{self.name}_{self.nsem}")
        self.nsem += 1
        self.count = 0

    def _waits(self, reads, writes):
        deps = []
        for b in reads:
            if b.w is not None:
                deps.append((b.w, True))
        for b in writes:
            if b.w is not None:
                deps.append((b.w, False))
            for t in b.r:
                deps.append((t, False))
        waits = []
        for t, raw in deps:
            if t.eng is self and (self.is_pe or (not raw and not self.strict)):
                continue
            k = id(t.sem)
            if self.waited.get(k, 0) >= t.val:
                continue
            self.waited[k] = t.val
            waits.append((t.sem, t.val))
        return waits

    def op(self, fn, reads=(), writes=(), inc=True):
        reads, writes = list(reads), list(writes)
        waits = self._waits(reads, writes)
        if not inc:
            self.items.append((waits, fn, None, 0))
            self.pend_r.extend(reads)
            self.pend_w.extend(writes)
            return None
        if self.count >= SEM_ROLL:
            self._roll()
        self.count += 1
        tok = Tok(self.sem, self.count, self)
        self.items.append((waits, fn, self.sem, 1))
        for b in reads + self.pend_r:
            b.r.append(tok)
        for b in writes + self.pend_w:
            b.w = tok
            b.r = []
        self.pend_r, self.pend_w = [], []
        return tok

    def dma(self, fn, dsem, reads=(), writes=()):
        reads, writes = list(reads), list(writes)
        waits = self._waits(reads, writes)
        dsem.count += 16
        tok = Tok(dsem.sem, dsem.count, None)
        self.items.append((waits, fn, dsem.sem, 16))
        for b in reads:
            b.r.append(tok)
        for b in writes:
            b.w = tok
            b.r = []
        return tok

    def wait_tok(self, tok):
        k = id(tok.sem)
        if self.waited.get(k, 0) >= tok.val:
            return
        self.waited[k] = tok.val
        self.items.append(([(tok.sem, tok.val)], None, None, 0))

    def replay(self, eng):
        for waits, fn, sem, n in self.items:
            for s, v in waits:
                eng.wait_ge(s, v)
            if fn is None:
                continue
            ins = fn(eng)
            if sem is not None:
                ins.then_inc(sem, n)


class Prog:
    def __init__(self, nc):
        self.nc = nc
        self.dsems = []
        self.pe = Eng(self, "pe", is_pe=True)
        self.act = Eng(self, "act", strict=True)
        self.dve = Eng(self, "dve", strict=True)
        self.pool = Eng(self, "pool", strict=True)
        self.sp = Eng(self, "sp")
        self.engs = [self.pe, self.act, self.dve, self.pool, self.sp]
        self.regcache = {}

    def bc_reg(self, eng, val):
        if val not in self.regcache:
            self.regcache[val] = eng.to_reg(val)
        return self.regcache[val]

    def barrier(self):
        toks = []
        for e in self.engs:
            assert not e.pend_r and not e.pend_w
            if e.count > 0:
                toks.append(Tok(e.sem, e.count, e))
        for d in self.dsems:
            if d.count > 0:
                toks.append(Tok(d.sem, d.count, None))
        for e in self.engs:
            for t in toks:
                if t.eng is e:
                    continue
                e.wait_tok(t)

    def emit(self):
        with self.nc.Block() as block:
            block.tensor(self.pe.replay)
            block.scalar(self.act.replay)
            block.vector(self.dve.replay)
            block.gpsimd(self.pool.replay)
            block.sync(self.sp.replay)
        for e in self.engs:
            e.items = []
        self.regcache = {}


D = 1024
NLOC = 4096
NMAIN = 2048
BLK = 512
NBLK = 8
ALPHA = float(2 ** 0.25)
LN_EPS = 1e-5
RMS_EPS = 1e-6
NEXP = 64
WINS = (2, 4, 8, 16)
CAP = 384
NROWS = NEXP * CAP
I32 = mybir.dt.int32
U32 = mybir.dt.uint32


def build(debug=None, n_exp=NEXP):
    nc = bass.Bass("TRN2", target_bir_lowering=False)

    def din(name, shape, dt=F32):
        return nc.dram_tensor(name, list(shape), dt, kind="ExternalInput").ap()

    def dout(name, shape, dt=F32):
        return nc.dram_tensor(name, list(shape), dt, kind="ExternalOutput").ap()

    xloc = din("xloc", [NLOC, D])
    w_in = din("w_in", [D, 2560])
    lb_logits = din("hg_lb_logits", [2, 512])
    hg_norm_g = din("hg_norm_g", [512])
    w_pool = din("w_pool", [4, 128, 128])
    pool_scale = din("pool_scale", [512])
    w_out = din("w_out", [D, D])
    ln1_g = din("ln1_g", [D]); ln1_b = din("ln1_b", [D])
    w_router = din("w_router", [D, 64])
    router_bias = din("router_bias", [64])
    if debug is None:
        w_gate = din("w_gate", [64, D, 256]); w_up = din("w_up", [64, D, 256]); w_down = din("w_down", [64, 256, D])
        ws_gate = din("ws_gate", [D, 256]); ws_up = din("ws_up", [D, 256]); ws_down = din("ws_down", [256, D])
        ln2_g = din("ln2_g", [D]); ln2_b = din("ln2_b", [D])
    c_identb = din("c_identb", [128, 128], BF16)
    c_identf = din("c_identf", [128, 128])
    c_cmask = din("c_cmask", [64, 64])
    c_rmask = din("c_rmask", [128, 512])
    c_invcnt = din("c_invcnt", [128, 4, 16])
    c_tri = din("c_tri", [128, 128], BF16)
    c_iota = din("c_iota", [128, 64])
    c_ecoff = din("c_ecoff", [128, 64])
    xg_dram = nc.dram_tensor("xg_dram", [NROWS, D], BF16, kind="Internal")
    y_dram = nc.dram_tensor("y_dram", [NROWS, D], BF16, kind="Internal")
    out = dout("out", [NMAIN, D])
    dbg = {}
    if debug == "mixer":
        dbg["mixT"] = dout("dbg_mixT", [128, 8, NMAIN], BF16)
    if debug in ("p4", "p4a", "p4b"):
        dbg["h1"] = dout("dbg_h1", [NMAIN, D])
        dbg["gw"] = dout("dbg_gw", [NMAIN, 64])

    P = Prog(nc)
    pe, act, dve, pool, sp = P.pe, P.act, P.dve, P.pool, P.sp
    glob = ExitStack()

    def sbt(es, name, shape, dt):
        return es.enter_context(nc.sbuf_tensor(name, list(shape), dt))

    def A(out_, in_, func, reads, writes, **kw):
        return act.op(lambda e: e.activation(out=out_, in_=in_, func=func, **kw), reads, writes)

    def TT(E, out_, in0, in1, op, reads, writes):
        return E.op(lambda e: e.tensor_tensor(out=out_, in0=in0, in1=in1, op=op), reads, writes)

    def TS(E, out_, in0, s1, s2, op0, op1, reads, writes, **kw):
        return E.op(lambda e: e.tensor_scalar(out=out_, in0=in0, scalar1=s1, scalar2=s2, op0=op0, op1=op1, **kw), reads, writes)

    def STT(out_, in0, scalar, in1, op0, op1, reads, writes, **kw):
        return dve.op(lambda e: e.scalar_tensor_tensor(out=out_, in0=in0, scalar=scalar, in1=in1, op0=op0, op1=op1, **kw), reads, writes)

    def CP(E, out_, in_, reads, writes):
        if E is act:
            return act.op(lambda e: e.activation(out=out_, in_=in_, func=AF.Copy), reads, writes)
        return E.op(lambda e: e.tensor_copy(out=out_, in_=in_), reads, writes)

    def MM(out_, lhsT, rhs, start, stop, reads, writes, inc):
        return pe.op(lambda e: e.matmul(out_, lhsT=lhsT, rhs=rhs, start=start, stop=stop), reads, writes, inc=inc)

    def TR(out_, in_, ident, reads, writes, inc):
        return pe.op(lambda e: e.transpose(out=out_, in_=in_, identity=ident), reads, writes, inc=inc)

    _bufsem = {}

    def DMA(E, out_, in_, dsem, reads=(), writes=(), **kw):
        reads, writes = list(reads), list(writes)
        key = writes[0] if writes else None
        if key is not None:
            if id(key) not in _bufsem:
                _bufsem[id(key)] = DSem(P, "dq_" + key.name)
            dsem = _bufsem[id(key)]
        return E.dma(lambda e: e.dma_start(out=out_, in_=in_, **kw), dsem, reads, writes)

    with glob:
        pb = [glob.enter_context(nc.psum_tensor(f"pb{i}", [128, 512], F32)) for i in range(8)]
        BP = [Buf(f"pb{i}") for i in range(8)]

        identb = sbt(glob, "identb", [128, 128], BF16); B_identb = Buf("identb")
        onesb = sbt(glob, "onesb", [128, 128], BF16); B_onesb = Buf("onesb")
        cmask = sbt(glob, "cmask", [64, 64], F32); B_cmask = Buf("cmask")
        rmask = sbt(glob, "rmask", [128, 512], F32); B_rmask = Buf("rmask")
        invcnt = sbt(glob, "invcnt", [128, 4, 16], F32); B_invcnt = Buf("invcnt")
        lbl = sbt(glob, "lbl", [128, 2, 4], F32); B_lbl = Buf("lbl")
        lbv = sbt(glob, "lbv", [128, 4], F32); B_lbv = Buf("lbv")
        omlv = sbt(glob, "omlv", [128, 4], F32); B_omlv = Buf("omlv")
        hgn = sbt(glob, "hgn", [128, 4], F32); B_hgn = Buf("hgn")
        pscale = sbt(glob, "pscale", [128, 4], F32); B_pscale = Buf("pscale")
        gw_all = sbt(glob, "gw_all", [128, 16, 64], F32); B_gw = [Buf(f"gw{i}") for i in range(16)]
        w8_all = sbt(glob, "w8_all", [128, 16, 8], F32); B_w8 = [Buf(f"w8_{i}") for i in range(16)]
        dest8 = sbt(glob, "dest8", [128, 128], I32); B_d8 = [Buf(f"d8_{i}") for i in range(16)]
        tri = sbt(glob, "tri", [128, 128], BF16); B_tri = Buf("tri")
        iota64 = sbt(glob, "iota64", [128, 64], F32); B_iota = Buf("iota64")
        ecoff = sbt(glob, "ecoff", [128, 64], F32); B_ecoff = Buf("ecoff")
        B_xg = Buf("xg_dram"); B_yd = Buf("y_dram")
        mixT = sbt(glob, "mixT", [128, 8, NMAIN], BF16)
        B_mix = [[Buf(f"mix{j}_{n}") for n in range(4)] for j in range(8)]

        ds_c = DSem(P, "ds_const")
        DMA(sp, identb[:], c_identb, ds_c, writes=[B_identb])
        DMA(sp, cmask[:], c_cmask, ds_c, writes=[B_cmask])
        DMA(sp, rmask[:], c_rmask, ds_c, writes=[B_rmask])
        DMA(sp, invcnt[:], c_invcnt, ds_c, writes=[B_invcnt])
        DMA(sp, tri[:], c_tri, ds_c, writes=[B_tri])
        DMA(sp, iota64[:], c_iota, ds_c, writes=[B_iota])
        DMA(sp, ecoff[:], c_ecoff, ds_c, writes=[B_ecoff])
        DMA(sp, lbl[:], lb_logits.rearrange("s (h p) -> p s h", p=128), ds_c, writes=[B_lbl], allow_slow_non_contiguous=True)
        DMA(sp, hgn[:], hg_norm_g.rearrange("(h p) -> p h", p=128), ds_c, writes=[B_hgn], allow_slow_non_contiguous=True)
        DMA(sp, pscale[:], pool_scale.rearrange("(h p) -> p h", p=128), ds_c, writes=[B_pscale], allow_slow_non_contiguous=True)
        dve.op(lambda e: e.memset(onesb[:], 1.0), [], [B_onesb])
        zrow = sbt(glob, "zrow", [128, D], BF16); B_zrow = Buf("zrow")
        dve.op(lambda e: e.memset(zrow[:], 0.0), [], [B_zrow])
        ds_zf = DSem(P, "ds_zfill")
        xg_v = xg_dram.ap().rearrange("(p r) d -> p r d", p=128)
        RPP = NROWS // 128
        ZCH = 16

        def zero_fill(j):
            sp.dma(lambda en, j=j: en.dma_start(out=xg_v[:, j * ZCH:(j + 1) * ZCH, :],
                                                in_=zrow[:].rearrange("p (o d) -> p o d", o=1).to_broadcast([128, ZCH, D])),
                   ds_zf, reads=[B_zrow], writes=[])
        NZF = RPP // ZCH
        TT(dve, lbv[:], lbl[:, 0, :], lbl[:, 1, :], ALU.subtract, [B_lbl], [B_lbv])
        A(lbv[:], lbv[:], AF.Sigmoid, [B_lbv], [B_lbv])
        TS(dve, omlv[:], lbv[:], -1.0, 1.0, ALU.mult, ALU.add, [B_lbv], [B_omlv])

        mix = ExitStack()
        with mix:
            w_in_sb = sbt(mix, "w_in_sb", [128, 8, 2560], BF16); B_win = [Buf(f"win{r}") for r in range(5)]
            wpool_sb = sbt(mix, "wpool_sb", [128, 4, 128], BF16); B_wpool = Buf("wpool")
            xb = [sbt(mix, f"xb{j}", [128, D], BF16) for j in range(4)]; B_xb = [Buf(f"xb{j}") for j in range(4)]
            ds_xb = [DSem(P, f"ds_xb{j}") for j in range(4)]
            xTb = sbt(mix, "xTb", [128, 8, BLK], BF16); B_xT = [Buf(f"xT{j}") for j in range(4)]
            vblk = sbt(mix, "vblk", [64, 8, 512], BF16); B_v = [Buf(f"v{c}") for c in range(8)]
            ubuf = sbt(mix, "ubuf", [128, 4, 528], F32); B_u = [Buf(f"u{g}") for g in range(4)]
            pp0 = sbt(mix, "pp0", [128, 528], F32); B_pp0 = Buf("pp0")
            pp1 = sbt(mix, "pp1", [128, 528], F32); B_pp1 = Buf("pp1")
            pfix = sbt(mix, "pfix", [128, 16], F32); B_pfix = Buf("pfix")
            pT = sbt(mix, "pT", [128, 4, 512], BF16); B_pT = [Buf(f"pT{g}") for g in range(4)]
            S_f = sbt(mix, "S_f", [128, 4, 128], F32); B_Sf = [Buf(f"Sf{h}") for h in range(4)]
            S_bf = sbt(mix, "S_bf", [128, 4, 8, 128], BF16); B_Sbf = [[Buf(f"Sbf{h}_{s}") for s in range(8)] for h in range(4)]
            NSET = 4
            names32 = ["sgf", "logf", "b", "eb", "sgT"]
            names16 = ["qt", "kt", "kdT"]
            T = [dict() for _ in range(NSET)]
            BT = [dict() for _ in range(NSET)]
            for s in range(NSET):
                for nm in names32:
                    T[s][nm] = sbt(mix, f"T{s}_{nm}", [128, 512], F32); BT[s][nm] = Buf(f"T{s}_{nm}")
                for nm in names16:
                    T[s][nm] = sbt(mix, f"T{s}_{nm}", [128, 512], BF16); BT[s][nm] = Buf(f"T{s}_{nm}")
                T[s]["kd"] = sbt(mix, f"T{s}_kd", [64, 8, 128], BF16); BT[s]["kd"] = Buf(f"T{s}_kd")
                T[s]["scm"] = sbt(mix, f"T{s}_scm", [64, 8, 64], BF16); BT[s]["scm"] = Buf(f"T{s}_scm")

            ds_win = [DSem(P, f"ds_win{r}") for r in range(5)]

            def load_x(n):
                for j in range(4):
                    r0 = n * BLK + j * 128
                    DMA(pool, xb[j][:], xloc[r0:r0 + 128, :], ds_xb[j], writes=[B_xb[j]])

            load_x(0)
            for r in (1, 2, 0, 3, 4):
                DMA(pool, w_in_sb[:, :, r * 512:(r + 1) * 512],
                    w_in[:, r * 512:(r + 1) * 512].rearrange("(c p) n -> p c n", p=128), ds_win[r], writes=[B_win[r]])
            ds_wp = DSem(P, "ds_wpool")
            DMA(pool, wpool_sb[:], w_pool.rearrange("g c d -> c g d"), ds_wp, writes=[B_wpool])
            dve.op(lambda e: e.memset(S_f[:], 0.0), [], B_Sf)
            dve.op(lambda e: e.memset(S_bf[:], 0.0), [], [b for hb in B_Sbf for b in hb])
            dve.op(lambda e: e.memset(ubuf[:], 0.0), [], B_u)

            TRB = 0
            IPB = [1, 2, 3]
            ipc = [0]

            def next_ip():
                i = IPB[ipc[0] % 3]
                ipc[0] += 1
                return i

            UB = [4, 5]
            SCB = 6
            OB = 7
            trv = pb[TRB][:].bitcast(BF16).rearrange("p (c t) -> p c t", t=128)
            scv = pb[SCB][:64, :].rearrange("p (c t) -> p c t", t=64)

            for n in range(NBLK):
                is_main = n >= 4
                mb = n - 4
                for j in range(4):
                    for c in range(8):
                        TR(trv[:, c, :], xb[j][:, c * 128:(c + 1) * 128], identb[:], [B_xb[j], B_identb], [BP[TRB]], inc=(c == 7))
                    xt_tok = CP(dve if j % 2 == 0 else act, xTb[:, :, j * 128:(j + 1) * 128], trv, [BP[TRB]], [B_xT[j]])
                if n + 1 < NBLK:
                    load_x(n + 1)
                if n >= 2:
                    sp.wait_tok(xt_tok)
                    for j in range((n - 2) * NZF // (NBLK - 2), (n - 1) * NZF // (NBLK - 2)):
                        zero_fill(j)
                for c in range(8):
                    bi = next_ip()
                    for kc in range(8):
                        MM(pb[bi][:64, :], xTb[:, kc, c * 64:(c + 1) * 64], w_in_sb[:, kc, 1024:1536], kc == 0, kc == 7,
                           [B_xT[c // 2], B_win[2]], [BP[bi]], inc=(kc == 7))
                    CP(act, vblk[:, c, :], pb[bi][:64, :], [BP[bi]], [B_v[c]])
                if is_main or n == 3:
                    for g in range(4):
                        bi = next_ip()
                        for kc in range(8):
                            MM(pb[bi][:], w_in_sb[:, kc, 2048 + g * 128:2048 + (g + 1) * 128], xTb[:, kc, :], kc == 0, kc == 7,
                               B_xT + [B_win[4]], [BP[bi]], inc=(kc == 7))
                        CP(dve, ubuf[:, g, 16:528], pb[bi][:], [BP[bi]], [B_u[g]])
                HS = range(4)
                fb, qb, gb = {}, {}, {}
                for h in HS:
                    bi = next_ip(); fb[h] = bi
                    for kc in range(8):
                        MM(pb[bi][:], w_in_sb[:, kc, 512 + h * 128:512 + (h + 1) * 128], xTb[:, kc, :], kc == 0, kc == 7,
                           B_xT + [B_win[1]], [BP[bi]], inc=(kc == 7))
                    A(T[h]["sgf"][:], pb[bi][:], AF.Sigmoid, [BP[bi]], [BT[h]["sgf"]])
                for h in HS:
                    t, bt = T[h], BT[h]
                    TS(dve, t["sgf"][:], t["sgf"][:], omlv[:, h:h + 1], lbv[:, h:h + 1], ALU.mult, ALU.add,
                       [bt["sgf"], B_omlv, B_lbv], [bt["sgf"]])
                for h in HS:
                    t, bt = T[h], BT[h]
                    A(t["logf"][:], t["sgf"][:], AF.Ln, [bt["sgf"]], [bt["logf"]])
                for h in HS:
                    t, bt = T[h], BT[h]
                    dve.op(lambda e, t=t: e.tensor_tensor_scan(out=t["b"][:], data0=rmask[:], data1=t["logf"][:], initial=0.0,
                                                                op0=ALU.mult, op1=ALU.add),
                           [bt["logf"], B_rmask], [bt["b"]])
                    TS(pool, t["sgf"][:], t["sgf"][:], -1.0, 1.0, ALU.mult, ALU.add, [bt["sgf"]], [bt["sgf"]])
                for h in HS:
                    t, bt = T[h], BT[h]
                    A(t["eb"][:], t["b"][:], AF.Exp, [bt["b"]], [bt["eb"]])
                    A(t["logf"][:], t["b"][:], AF.Exp, [bt["b"]], [bt["logf"]], scale=-1.0)
                for h in HS:
                    t, bt = T[h], BT[h]
                    TT(dve, t["kt"][:], t["sgf"][:], t["logf"][:], ALU.mult, [bt["sgf"], bt["logf"]], [bt["kt"]])
                    eb3 = t["eb"][:].rearrange("p (c j) -> p c j", j=64)
                    TT(dve, t["kdT"][:].rearrange("p (c j) -> p c j", j=64), t["kt"][:].rearrange("p (c j) -> p c j", j=64),
                       eb3[:, :, 63:64].to_broadcast([128, 8, 64]), ALU.mult, [bt["kt"], bt["eb"]], [bt["kdT"]])
                if is_main:
                    for h in HS:
                        bi = next_ip()
                        for kc in range(8):
                            MM(pb[bi][:], w_in_sb[:, kc, h * 128:(h + 1) * 128], xTb[:, kc, :], kc == 0, kc == 7,
                               B_xT + [B_win[0]], [BP[bi]], inc=(kc == 7))
                        A(T[h]["b"][:], pb[bi][:], AF.Silu, [BP[bi]], [BT[h]["b"]])
                    for h in HS:
                        bi = next_ip()
                        for kc in range(8):
                            MM(pb[bi][:], w_in_sb[:, kc, 1536 + h * 128:1536 + (h + 1) * 128], xTb[:, kc, :], kc == 0, kc == 7,
                               B_xT + [B_win[3]], [BP[bi]], inc=(kc == 7))
                        A(T[h]["sgT"][:], pb[bi][:], AF.Silu, [BP[bi]], [BT[h]["sgT"]])
                    for h in HS:
                        t, bt = T[h], BT[h]
                        TT(dve, t["qt"][:], t["b"][:], t["eb"][:], ALU.mult, [bt["b"], bt["eb"]], [bt["qt"]])
                for h in HS:
                    t, bt = T[h], BT[h]
                    for c in range(8):
                        TR(trv[:64, c, :], t["kdT"][:, c * 64:(c + 1) * 64], identb[:], [bt["kdT"], B_identb], [BP[TRB]], inc=(c == 7))
                    CP(act, t["kd"][:], trv[:64, :, :], [BP[TRB]], [bt["kd"]])
                if is_main:
                    for h in HS:
                        t, bt = T[h], BT[h]
                        for c in range(8):
                            MM(scv[:, c, :], t["kt"][:, c * 64:(c + 1) * 64], t["qt"][:, c * 64:(c + 1) * 64], True, True,
                               [bt["kt"], bt["qt"]], [BP[SCB]], inc=(c == 7))
                        TT(dve, t["scm"][:], scv, cmask[:].rearrange("p (o t) -> p o t", o=1).to_broadcast([64, 8, 64]), ALU.mult,
                           [BP[SCB], B_cmask], [bt["scm"]])
                if is_main:
                    for g in range(4):
                        w = WINS[g]
                        src, Bsrc = ubuf[:, g, :], B_u[g]
                        d = 1
                        lvl = 0
                        while d < w:
                            dst, Bdst = (pp0, B_pp0) if lvl % 2 == 0 else (pp1, B_pp1)
                            lo = 2 * d - 1
                            TT(pool, dst[:, lo:528], src[:, lo:528], src[:, lo - d:528 - d], ALU.add, [Bsrc], [Bdst])
                            src, Bsrc = dst[:], Bdst
                            d *= 2
                            lvl += 1
                        STT(pT[:, g, :], src[:, 16:528], 1.0 / w, ubuf[:, g, 16:528], ALU.mult, ALU.subtract, [Bsrc, B_u[g]], [B_pT[g]])
                        if mb == 0:
                            TT(dve, pfix[:], src[:, 16:32], invcnt[:, g, :], ALU.mult, [Bsrc, B_invcnt], [B_pfix])
                            TT(dve, pT[:, g, 0:16], pfix[:], ubuf[:, g, 16:32], ALU.subtract, [B_pfix, B_u[g]], [B_pT[g]])
                        bi = next_ip()
                        MM(pb[bi][:], wpool_sb[:, g, :], pT[:, g, :], True, True, [B_wpool, B_pT[g]], [BP[bi]], inc=True)
                        A(mixT[:, 4 + g, mb * BLK:(mb + 1) * BLK], pb[bi][:], AF.Copy, [BP[bi], B_pscale], [B_mix[4 + g][mb]], scale=pscale[:, g:g + 1])
                OBK = [OB] + IPB

                def emit_U(c):
                    ub = UB[c % 2]
                    for h in HS:
                        MM(pb[ub][:, h * 128:(h + 1) * 128], T[h]["kd"][:, c, :], vblk[:, c, h * 128:(h + 1) * 128], True, True,
                           [BT[h]["kd"], B_v[c]], [BP[ub]], inc=(h == 3))

                emit_U(0)
                for c in range(8):
                    if c + 1 < 8:
                        emit_U(c + 1)
                    if is_main:
                        for h in HS:
                            t, bt = T[h], BT[h]
                            MM(pb[OBK[h]][:, c * 64:(c + 1) * 64], S_bf[:, h, c, :], t["qt"][:, c * 64:(c + 1) * 64], True, False,
                               [B_Sbf[h][c], bt["qt"]], [BP[OBK[h]]], inc=False)
                            MM(pb[OBK[h]][:, c * 64:(c + 1) * 64], vblk[:, c, h * 128:(h + 1) * 128], t["scm"][:, c, :], False, True,
                               [B_v[c], bt["scm"]], [BP[OBK[h]]], inc=True)
                    ub = UB[c % 2]
                    for h in HS:
                        t, bt = T[h], BT[h]
                        STT(S_f[:, h, :], S_f[:, h, :], t["eb"][:, c * 64 + 63:c * 64 + 64], pb[ub][:, h * 128:(h + 1) * 128],
                            ALU.mult, ALU.add, [B_Sf[h], bt["eb"], BP[ub]], [B_Sf[h]])
                    nslot = (c + 1) % 8
                    if is_main or (n == 3 and c == 7):
                        for h in HS:
                            CP(act, S_bf[:, h, nslot, :], S_f[:, h, :], [B_Sf[h]], [B_Sbf[h][nslot]])
                if is_main:
                    for h in HS:
                        A(T[h]["kdT"][:], pb[OBK[h]][:], AF.Square, [BP[OBK[h]]], [BT[h]["kdT"]])
                    for h in HS:
                        bs = UB[h % 2]
                        MM(pb[bs][:], onesb[:], T[h]["kdT"][:], True, True, [B_onesb, BT[h]["kdT"]], [BP[bs]], inc=True)
                        A(T[h]["logf"][:], pb[bs][:], AF.Ln, [BP[bs]], [BT[h]["logf"]], scale=1.0 / 128.0, bias=RMS_EPS)
                    for h in HS:
                        A(T[h]["logf"][:], T[h]["logf"][:], AF.Exp, [BT[h]["logf"]], [BT[h]["logf"]], scale=-0.5)
                    for h in HS:
                        t, bt = T[h], BT[h]
                        TT(dve, t["sgf"][:], pb[OBK[h]][:], t["logf"][:], ALU.mult, [BP[OBK[h]], bt["logf"]], [bt["sgf"]])
                        STT(mixT[:, h, mb * BLK:(mb + 1) * BLK], t["sgf"][:], hgn[:, h:h + 1], t["sgT"][:], ALU.mult, ALU.mult,
                            [bt["sgf"], B_hgn, bt["sgT"]], [B_mix[h][mb]])
                if is_main or n == 3:
                    for g in range(4):
                        CP(pool, ubuf[:, g, 0:16], ubuf[:, g, 512:528], [B_u[g]], [B_u[g]])

            if debug == "mixer":
                ds_dbg = DSem(P, "ds_dbg")
                tk = DMA(sp, dbg["mixT"], mixT[:], ds_dbg, reads=[b for row in B_mix for b in row])
                sp.wait_tok(tk)
                P.emit()
                return nc
            P.barrier()
            P.emit()

        post = ExitStack()
        with post:
            h1 = sbt(post, "h1", [128, 16, D], F32); B_h1 = [[Buf(f"h1_{i}_{hf}") for hf in range(2)] for i in range(16)]
            h1T = sbt(post, "h1T", [128, 8, NMAIN], BF16); B_h1T = [Buf(f"h1T{i}") for i in range(16)]
            lng = sbt(post, "lng", [128, D], F32); B_lng = Buf("lng")
            lnb = sbt(post, "lnb", [128, D], F32); B_lnb = Buf("lnb")
            ds_ln = DSem(P, "ds_ln")
            DMA(sp, lng[:], ln1_g.partition_broadcast(128), ds_ln, writes=[B_lng])
            DMA(sp, lnb[:], ln1_b.partition_broadcast(128), ds_ln, writes=[B_lnb])
            stats = sbt(post, "stats", [128, 2, 6], F32); B_stats = Buf("stats")
            mv = sbt(post, "mv", [128, 2], F32); B_mv = Buf("mv")
            rstd = sbt(post, "rstd", [128, 1], F32); B_rstd = Buf("rstd")

            nmr = sbt(post, "nmr", [128, 1], F32); B_nmr = Buf("nmr")

            def layer_norm(src, Bsrc_list, dst, Bdst_list, eng_b=pool, on_act=False):
                for hf in range(2):
                    dve.op(lambda e, hf=hf: e.bn_stats(out=stats[:, hf, :], in_=src[:, hf * 512:(hf + 1) * 512]), Bsrc_list, [B_stats])
                dve.op(lambda e: e.bn_aggr(out=mv[:], in_=stats[:].rearrange("p a b -> p (a b)")), [B_stats], [B_mv])
                A(rstd[:], mv[:, 1:2], AF.Ln, [B_mv], [B_rstd], bias=LN_EPS)
                A(rstd[:], rstd[:], AF.Exp, [B_rstd], [B_rstd], scale=-0.5)
                if on_act:
                    TS(dve, nmr[:], mv[:, 0:1], rstd[:, 0:1], -1.0, ALU.mult, ALU.mult, [B_mv, B_rstd], [B_nmr])
                    A(dst, src, AF.Identity, Bsrc_list + [B_rstd, B_nmr], Bdst_list, scale=rstd[:, 0:1], bias=nmr[:, 0:1])
                else:
                    TS(dve, dst, src, mv[:, 0:1], rstd[:, 0:1], ALU.subtract, ALU.mult, Bsrc_list + [B_mv, B_rstd], Bdst_list)
                TT(dve, dst, dst, lng[:], ALU.mult, Bdst_list + [B_lng], Bdst_list)
                TT(eng_b, dst, dst, lnb[:], ALU.add, Bdst_list + [B_lnb], Bdst_list)

            p4 = ExitStack()
            with p4:
                wout_sb = sbt(p4, "wout_sb", [128, 8, D], BF16); B_wout = Buf("wout")
                wr_sb = sbt(p4, "wr_sb", [128, 8, 64], F32); B_wr = Buf("wr")
                rbias = sbt(p4, "rbias", [128, 64], F32); B_rbias = Buf("rbias")
                xres = [sbt(p4, f"xres{i}", [128, D], F32) for i in range(2)]; B_xres = [Buf(f"xres{i}") for i in range(2)]
                ds_xres = [DSem(P, f"ds_xres{i}") for i in range(2)]
                rres = sbt(p4, "rres", [128, D], F32); B_rres = Buf("rres")
                hhi2 = [sbt(p4, f"hhi{q}", [128, D], BF16) for q in range(2)]; B_hhi2 = [Buf(f"hhi{q}") for q in range(2)]
                maskb = sbt(p4, "maskb", [128, 16, 64], BF16); B_mask = [Buf(f"mask{i}") for i in range(16)]
                hlo = sbt(p4, "hlo", [128, D], BF16); B_hlo = Buf("hlo")
                loT = sbt(p4, "loT", [128, 8, 128], BF16); B_loT = Buf("loT")
                whi = sbt(p4, "whi", [128, 8, 64], BF16); B_whi = Buf("whi")
                wlo = sbt(p4, "wlo", [128, 8, 64], BF16); B_wlo = Buf("wlo")
                sc_all = sbt(p4, "sc_all", [128, 16, 64], F32); B_sc = [Buf(f"sc{i}") for i in range(16)]
                cumb = sbt(p4, "cumb", [128, 17, 64], BF16); B_cum = [Buf(f"cum{i}") for i in range(17)]
                hhiB = [sbt(p4, f"hhiB{q}", [128, D], BF16) for q in range(2)]; B_hhiB = [Buf(f"hhiB{q}") for q in range(2)]
                G = 4
                rt = {}
                Brt = {}
                for nm, shp in [("sel", [128, G * 64]), ("eq", [128, G * 64]), ("sel2", [128, G * 64]), ("selm", [128, G * 64]), ("w", [128, G * 64]),
                                ("dall", [128, G * 64]),
                                ("m1", [128, G * 8]), ("m2", [128, G * 8]), ("gs", [128, G * 8]), ("g8", [128, G * 8]), ("pen", [128, G * 8]),
                                ("t8", [128, G * 8]), ("i8f", [128, G * 8]), ("d8f", [128, G * 8]), ("okm", [128, G * 8]),
                                ("wsum", [128, G]), ("rs", [128, G])]:
                    rt[nm] = sbt(p4, f"rt_{nm}", shp, F32); Brt[nm] = Buf(f"rt_{nm}")
                for new_nm, old_nm in (("oh", "eq"), ("junk", "sel2"), ("ovf", "selm")):
                    rt[new_nm], Brt[new_nm] = rt[old_nm], Brt[old_nm]
                rt_i8 = sbt(p4, "rt_i8", [128, G * 8], U32); B_i8 = Buf("rt_i8")
                ds_w4 = DSem(P, "ds_w4")
                DMA(pool, wout_sb[:], w_out.rearrange("(c p) n -> p c n", p=128), ds_w4, writes=[B_wout])
                DMA(sp, wr_sb[:], w_router.rearrange("(c p) n -> p c n", p=128), ds_ln, writes=[B_wr])
                DMA(sp, rbias[:], router_bias.partition_broadcast(128), ds_ln, writes=[B_rbias])
                CP(dve, whi[:], wr_sb[:], [B_wr], [B_whi])
                TT(dve, wlo[:], wr_sb[:], whi[:], ALU.subtract, [B_wr, B_whi], [B_wlo])
                dve.op(lambda e: e.memset(cumb[:, 0, :], 0.0), [], [B_cum[0]])

                ds_push = [DSem(P, f"ds_push{k}") for k in range(16)]

                def load_xres(i):
                    DMA(sp, xres[i % 2][:], xloc[NMAIN + i * 128:NMAIN + (i + 1) * 128, :], ds_xres[i % 2], writes=[B_xres[i % 2]])

                def bc3(ap2, n_mid, n_in):
                    return ap2.rearrange("p (g o) -> p g o", o=1).to_broadcast([128, n_mid, n_in])

                def stage_a1(i):
                    blk = i // 4
                    for hf in range(2):
                        for j in range(8):
                            MM(pb[hf][:], mixT[:, j, i * 128:(i + 1) * 128], wout_sb[:, j, hf * 512:(hf + 1) * 512], j == 0, j == 7,
                               [B_mix[j][blk], B_wout], [BP[hf]], inc=(j == 7))
                        STT(rres[:, hf * 512:(hf + 1) * 512], xres[i % 2][:, hf * 512:(hf + 1) * 512], ALPHA, pb[hf][:], ALU.mult, ALU.add,
                            [B_xres[i % 2], BP[hf]], [B_rres])
                    layer_norm(rres[:], [B_rres], h1[:, i, :], B_h1[i], eng_b=dve)

                def stage_a2(i):
                    hhi, B_hhi = hhi2[i % 2], B_hhi2[i % 2]
                    CP(act, hhi[:], h1[:, i, :], B_h1[i], [B_hhi])
                    TT(dve, hlo[:], h1[:, i, :], hhi[:], ALU.subtract, B_h1[i] + [B_hhi], [B_hlo])
                    tv2 = pb[2][:].bitcast(BF16).rearrange("p (c t) -> p c t", t=128)
                    tv3 = pb[3][:].bitcast(BF16).rearrange("p (c t) -> p c t", t=128)
                    for c in range(8):
                        TR(tv2[:, c, :], hhi[:, c * 128:(c + 1) * 128], identb[:], [B_hhi, B_identb], [BP[2]], inc=(c == 7))
                    for c in range(8):
                        TR(tv3[:, c, :], hlo[:, c * 128:(c + 1) * 128], identb[:], [B_hlo, B_identb], [BP[3]], inc=(c == 7))
                    CP(act, h1T[:, :, i * 128:(i + 1) * 128], tv2, [BP[2]], [B_h1T[i]])
                    CP(dve, loT[:], tv3, [BP[3]], [B_loT])
                    nmm = 0
                    for (lt, Bl, rw, Br) in ((h1T, B_h1T[i], whi, B_whi), (None, B_loT, whi, B_whi), (h1T, B_h1T[i], wlo, B_wlo)):
                        for c in range(8):
                            lhs = loT[:, c, :] if lt is None else h1T[:, c, i * 128:(i + 1) * 128]
                            MM(pb[4][:, 0:64], lhs, rw[:, c, :], nmm == 0, nmm == 23, [Bl, Br], [BP[4]], inc=(nmm == 23))
                            nmm += 1
                    A(sc_all[:, i, :], pb[4][:, 0:64], AF.Exp, [BP[4]], [B_sc[i]], scale=-1.0)
                    TS(dve, sc_all[:, i, :], sc_all[:, i, :], 1.0, None, ALU.add, ALU.bypass, [B_sc[i]], [B_sc[i]])
                    dve.op(lambda e, i=i: e.reciprocal(out=sc_all[:, i, :], in_=sc_all[:, i, :]), [B_sc[i]], [B_sc[i]])

                def stage_b(g):
                    tl = list(range(G * g, G * g + G))
                    Bsel = [Brt["sel"]]
                    Bsc = [B_sc[i] for i in tl]
                    Bgw = [B_gw[i] for i in tl]
                    sc2d = sc_all[:, G * g:G * g + G, :].rearrange("p t e -> p (t e)")
                    sel2d = rt["sel"][:]
                    sel44 = sel2d.rearrange("p (q e) -> p q e", e=8)
                    TT(dve, sel2d.rearrange("p (t e) -> p t e", e=64), sc_all[:, G * g:G * g + G, :],
                       rbias[:].rearrange("p (o e) -> p o e", o=1).to_broadcast([128, G, 64]), ALU.add, Bsc + [B_rbias], Bsel)
                    gw2d = gw_all[:, G * g:G * g + G, :].rearrange("p t e -> p (t e)")
                    NQ = G * 8
                    dve.op(lambda e: e.tensor_reduce(out=rt["m1"][:], in_=sel44, axis=AX.X, op=ALU.max), Bsel, [Brt["m1"]])
                    TT(dve, rt["eq"][:].rearrange("p (q e) -> p q e", e=8), sel44, bc3(rt["m1"][:], NQ, 8), ALU.is_equal, Bsel + [Brt["m1"]], [Brt["eq"]])
                    STT(rt["sel2"][:], rt["eq"][:], -1.0e9, sel2d, ALU.mult, ALU.add, [Brt["eq"]] + Bsel, [Brt["sel2"]])
                    dve.op(lambda e: e.tensor_reduce(out=rt["m2"][:], in_=rt["sel2"][:].rearrange("p (q e) -> p q e", e=8), axis=AX.X, op=ALU.max),
                           [Brt["sel2"]], [Brt["m2"]])
                    TT(dve, rt["gs"][:], rt["m1"][:], rt["m2"][:], ALU.add, [Brt["m1"], Brt["m2"]], [Brt["gs"]])
                    for t in range(G):
                        dve.op(lambda e, t=t: e.max(out=rt["g8"][:, t * 8:(t + 1) * 8], in_=rt["gs"][:, t * 8:(t + 1) * 8]), [Brt["gs"]], [Brt["g8"]])
                    g83 = rt["g8"][:].rearrange("p (t k) -> p t k", k=8)
                    TT(dve, rt["pen"][:].rearrange("p (t k) -> p t k", k=8), rt["gs"][:].rearrange("p (t k) -> p t k", k=8),
                       g83[:, :, 3:4].to_broadcast([128, G, 8]), ALU.is_lt, [Brt["gs"], Brt["g8"]], [Brt["pen"]])
                    TS(dve, rt["pen"][:], rt["pen"][:], -1.0e9, None, ALU.mult, ALU.bypass, [Brt["pen"]], [Brt["pen"]])
                    TT(dve, rt["selm"][:].rearrange("p (q e) -> p q e", e=8), sel44, bc3(rt["pen"][:], NQ, 8), ALU.add, Bsel + [Brt["pen"]], [Brt["selm"]])
                    for t in range(G):
                        dve.op(lambda e, t=t: e.max(out=rt["t8"][:, t * 8:(t + 1) * 8], in_=rt["selm"][:, t * 64:(t + 1) * 64]), [Brt["selm"]], [Brt["t8"]])
                    t83 = rt["t8"][:].rearrange("p (t k) -> p t k", k=8)
                    selm3 = rt["selm"][:].rearrange("p (t e) -> p t e", e=64)
                    w3 = rt["w"][:].rearrange("p (t e) -> p t e", e=64)
                    TT(dve, w3, selm3, t83[:, :, 7:8].to_broadcast([128, G, 64]), ALU.is_ge, [Brt["selm"], Brt["t8"]], [Brt["w"]])
                    TT(dve, rt["w"][:], rt["w"][:], sc2d, ALU.mult, [Brt["w"]] + Bsc, [Brt["w"]])
                    dve.op(lambda e: e.tensor_reduce(out=rt["wsum"][:], in_=w3, axis=AX.X, op=ALU.add), [Brt["w"]], [Brt["wsum"]])
                    dve.op(lambda e: e.reciprocal(out=rt["rs"][:], in_=rt["wsum"][:]), [Brt["wsum"]], [Brt["rs"]])
                    STT(gw_all[:, G * g:G * g + G, :], w3, 2.5, bc3(rt["rs"][:], G, 64), ALU.mult, ALU.mult, [Brt["w"], Brt["rs"]], Bgw)
                    if debug is not None:
                        return
                    yield
                    for t, i in enumerate(tl):
                        TS(dve, maskb[:, i, :], gw_all[:, i, :], 0.0, None, ALU.is_gt, ALU.bypass, [B_gw[i]], [B_mask[i]])
                        TT(dve, cumb[:, i + 1, :], cumb[:, i, :], maskb[:, i, :], ALU.add, [B_cum[i], B_mask[i]], [B_cum[i + 1]])
                        MM(pb[5][:, t * 64:(t + 1) * 64], tri[:], maskb[:, i, :], True, False, [B_tri, B_mask[i]], [BP[5]], inc=False)
                        MM(pb[5][:, t * 64:(t + 1) * 64], onesb[:], cumb[:, i, :], False, True, [B_onesb, B_cum[i]], [BP[5]], inc=(t == G - 1))
                    ec3 = ecoff[:].rearrange("p (o e) -> p o e", o=1).to_broadcast([128, G, 64])
                    io3 = iota64[:].rearrange("p (o e) -> p o e", o=1).to_broadcast([128, G, 64])
                    pos3 = pb[5][:, 0:G * 64].rearrange("p (t e) -> p t e", e=64)
                    dall3 = rt["dall"][:].rearrange("p (t e) -> p t e", e=64)
                    TT(dve, dall3, pos3, ec3, ALU.add, [BP[5], B_ecoff], [Brt["dall"]])
                    TS(dve, rt["ovf"][:], pb[5][:, 0:G * 64], float(CAP), 1.0e6, ALU.is_ge, ALU.mult, [BP[5]], [Brt["ovf"]])
                    TT(dve, rt["dall"][:], rt["dall"][:], rt["ovf"][:], ALU.add, [Brt["dall"], Brt["ovf"]], [Brt["dall"]])
                    yield
                    for t, i in enumerate(tl):
                        dve.op(lambda e, i=i: e.max(out=w8_all[:, i, :], in_=gw_all[:, i, :]), [B_gw[i]], [B_w8[i]])
                        dve.op(lambda e, i=i, t=t: e.max_index(out=rt_i8[:, t * 8:(t + 1) * 8], in_max=w8_all[:, i, :], in_values=gw_all[:, i, :]),
                               [B_gw[i], B_w8[i]], [B_i8])
                    CP(dve, rt["i8f"][:], rt_i8[:], [B_i8], [Brt["i8f"]])
                    i83 = rt["i8f"][:].rearrange("p (t k) -> p t k", k=8)
                    d83 = rt["d8f"][:].rearrange("p (t k) -> p t k", k=8)
                    oh3 = rt["oh"][:].rearrange("p (t e) -> p t e", e=64)
                    jk3 = rt["junk"][:].rearrange("p (t e) -> p t e", e=64)
                    for k in range(8):
                        TT(dve, oh3, io3, i83[:, :, k:k + 1].to_broadcast([128, G, 64]), ALU.is_equal, [B_iota, Brt["i8f"]], [Brt["oh"]])
                        TT(dve, rt["junk"][:], rt["oh"][:], rt["dall"][:], ALU.mult, [Brt["oh"], Brt["dall"]], [Brt["junk"]])
                        dve.op(lambda e, k=k: e.tensor_reduce(out=d83[:, :, k], in_=jk3, axis=AX.X, op=ALU.add), [Brt["junk"]], [Brt["d8f"]])
                    CP(dve, dest8[:, G * g * 8:(G * g + G) * 8], rt["d8f"][:], [Brt["d8f"]], [B_d8[i] for i in tl])
                    TS(dve, rt["okm"][:], rt["d8f"][:], float(NROWS), None, ALU.is_lt, ALU.bypass, [Brt["d8f"]], [Brt["okm"]])
                    w8g = w8_all[:, G * g:G * g + G, :].rearrange("p t k -> p (t k)")
                    TT(dve, w8g, w8g, rt["okm"][:], ALU.mult, [B_w8[i] for i in tl] + [Brt["okm"]], [B_w8[i] for i in tl])
                    yield
                    for t, i in enumerate(tl):
                        hb_, Bhb = hhiB[i % 2], B_hhiB[i % 2]
                        CP(act, hb_[:].rearrange("t (c p) -> t c p", p=128), h1[:, i, :].rearrange("t (p c) -> t c p", c=8), B_h1[i], [Bhb])
                        for k in range(8):
                            pool.dma(lambda e, i=i, k=k, hb_=hb_: e.indirect_dma_start(
                                out=xg_dram[:, :], out_offset=bass.IndirectOffsetOnAxis(ap=dest8[:, i * 8 + k:i * 8 + k + 1], axis=0), in_=hb_[:], in_offset=None,
                                bounds_check=P.bc_reg(e, NROWS - 1), oob_is_err=False), ds_push[(i % 2) * 8 + k], reads=[Bhb, B_d8[i]], writes=[])

                def drain(gen):
                    for _ in gen:
                        pass

                load_xres(0)
                stage_a1(0)
                pending = None
                for i in range(16):
                    if i + 1 < 16:
                        load_xres(i + 1)
                        stage_a1(i + 1)
                    if debug == "p4a":
                        continue
                    stage_a2(i)
                    if pending is not None:
                        if next(pending, "done") == "done":
                            pending = None
                    if i % G == G - 1:
                        if pending is not None:
                            drain(pending)
                        pending = stage_b(i // G)
                if pending is not None:
                    drain(pending)

                if debug in ("p4", "p4a", "p4b"):
                    ds_dbg = DSem(P, "ds_dbg")
                    tk = DMA(sp, dbg["h1"].rearrange("(i p) d -> p i d", p=128), h1[:], ds_dbg, reads=[b for r in B_h1 for b in r])
                    if debug != "p4a":
                        tk = DMA(sp, dbg["gw"].rearrange("(i p) e -> p i e", p=128), gw_all[:], ds_dbg, reads=B_gw)
                    sp.wait_tok(tk)
                    P.emit()
                    return nc
                P.barrier()
                P.emit()

            moe = ExitStack()
            with moe:
                NWB = 2
                wg = [sbt(moe, f"wg{i}", [128, 8, 256], BF16) for i in range(NWB)]
                wu = [sbt(moe, f"wu{i}", [128, 8, 256], BF16) for i in range(NWB)]
                wd = [sbt(moe, f"wd{i}", [128, 2, D], BF16) for i in range(NWB)]
                B_wg = [Buf(f"wg{i}") for i in range(NWB)]; B_wu = [Buf(f"wu{i}") for i in range(NWB)]; B_wd = [Buf(f"wd{i}") for i in range(NWB)]
                xs = [sbt(moe, f"xs{i}", [128, D], BF16) for i in range(4)]; B_xs = [Buf(f"xs{i}") for i in range(4)]
                flat = mixT[:].rearrange("p a b -> p (a b)")
                xgT = [flat[:, i * 8 * CAP:(i + 1) * 8 * CAP].rearrange("p (c t) -> p c t", t=CAP) for i in range(2)]
                B_xgT = [[Buf(f"xgT{i}_{q}") for q in range(CAP // 128)] for i in range(2)]
                sl = [sbt(moe, f"sl{i}", [128, 512], BF16) for i in range(2)]; B_sl = [Buf(f"sl{i}") for i in range(2)]
                hT = [sbt(moe, f"hT{i}", [128, 2, 512], BF16) for i in range(2)]; B_hT = [[Buf(f"hT{i}_{hc}") for hc in range(2)] for i in range(2)]
                ysb = [flat[:, 12288 + i * D:12288 + (i + 1) * D] for i in range(3)]; B_ysb = [Buf(f"ysb{i}") for i in range(3)]
                NYK = 8
                yk = [flat[:, 8192 + i * D:8192 + (i + 1) * D] for i in range(4)] + [sbt(moe, f"yk{i}", [128, D], BF16) for i in range(4, NYK)]
                B_yk = [Buf(f"yk{i}") for i in range(NYK)]
                ostg = [sbt(moe, f"ostg{i}", [128, D], F32) for i in range(2)]; B_ostg = [Buf(f"ostg{i}") for i in range(2)]
                ds_out = [DSem(P, f"ds_out{i}") for i in range(2)]
                ds_yst = [DSem(P, f"ds_yst{i}") for i in range(3)]
                ds_pull = [DSem(P, f"ds_pull{i}") for i in range(8)]
                DMA(sp, lng[:], ln2_g.partition_broadcast(128), None, writes=[B_lng])
                DMA(sp, lnb[:], ln2_b.partition_broadcast(128), None, writes=[B_lnb])
                for q in range(NYK):
                    pool.op(lambda e, q=q: e.memset(yk[q][:], 0.0), [], [B_yk[q]])

                def load_expert(e):
                    s_ = e % NWB
                    if e < NEXP:
                        g_ap, u_ap, d_ap = w_gate[e], w_up[e], w_down[e]
                    else:
                        g_ap, u_ap, d_ap = ws_gate, ws_up, ws_down
                    lay = "(p c) n -> p c n" if e < NEXP else "(c p) n -> p c n"
                    DMA(pool, wg[s_][:], g_ap.rearrange(lay, p=128), None, writes=[B_wg[s_]])
                    DMA(pool, wu[s_][:], u_ap.rearrange(lay, p=128), None, writes=[B_wu[s_]])
                    DMA(pool, wd[s_][:], d_ap.rearrange("(c p) n -> p c n", p=128), None, writes=[B_wd[s_]])

                experts = list(range(n_exp)) + [NEXP]
                load_expert(experts[0])
                for i in range(16):
                    for hf in range(2):
                        A(h1[:, i, hf * 512:(hf + 1) * 512], h1[:, i, hf * 512:(hf + 1) * 512], AF.Copy, [B_h1[i][hf]], [B_h1[i][hf]], scale=ALPHA)
                TRBS = [0, 1]
                trvs = [pb[b_][:].bitcast(BF16).rearrange("p (c t) -> p c t", t=128) for b_ in TRBS]
                GUB = [2, 3, 4]
                guc = [0]
                YB = [5, 6, 7]
                yc = 0
                uidx = 0
                xsc = 0
                ysc = 0
                evc = 0
                NSB = CAP // 128

                def prep_loads(ei, e):
                    for q in range(NSB):
                        DMA(sp, xs[q][:], xg_dram[e * CAP + q * 128:e * CAP + (q + 1) * 128, :], None, writes=[B_xs[q]])

                def prep_transposes(ei, e):
                    xb_ = ei % 2
                    for q in range(NSB):
                        tb = TRBS[q % 2]
                        for c in range(8):
                            TR(trvs[q % 2][:, c, :], xs[q][:, c * 128:(c + 1) * 128], identb[:], [B_xs[q], B_identb], [BP[tb]], inc=(c == 7))
                        CP(act if q % 2 == 0 else dve, xgT[xb_][:, :, q * 128:(q + 1) * 128], trvs[q % 2], [BP[tb]], [B_xgT[xb_][q]])

                if experts[0] < NEXP:
                    prep_loads(0, experts[0])
                    prep_transposes(0, experts[0])
                for ei, e in enumerate(experts):
                    s_ = e % NWB
                    xb_ = ei % 2
                    nxt = experts[ei + 1] if ei + 1 < len(experts) else None
                    if nxt is not None:
                        load_expert(nxt)
                        if nxt < NEXP:
                            prep_loads(ei + 1, nxt)
                    nblk = 1 if e < NEXP else 4
                    W = CAP if e < NEXP else 512
                    for blk in range(nblk):
                        hb = uidx % 2
                        uidx += 1
                        for hc in range(2):
                            gu = []
                            for (wt, Bw) in ((wg, B_wg), (wu, B_wu)):
                                bk = GUB[guc[0] % 3]
                                guc[0] += 1
                                gu.append(bk)
                                for kc in range(8):
                                    if e < NEXP:
                                        rhs, Br = xgT[xb_][:, kc, :], B_xgT[xb_]
                                    else:
                                        rhs, Br = h1T[:, kc, blk * 512:(blk + 1) * 512], B_h1T[blk * 4:(blk + 1) * 4]
                                    MM(pb[bk][:, 0:W], wt[s_][:, kc, hc * 128:(hc + 1) * 128], rhs, kc == 0, kc == 7,
                                       [Bw[s_]] + Br, [BP[bk]], inc=(kc == 7))
                            A(sl[hc][:, 0:W], pb[gu[0]][:, 0:W], AF.Silu, [BP[gu[0]]], [B_sl[hc]])
                            TT(dve, hT[hb][:, hc, 0:W], sl[hc][:, 0:W], pb[gu[1]][:, 0:W], ALU.mult, [B_sl[hc], BP[gu[1]]], [B_hT[hb][hc]])
                        if blk == 0 and nxt is not None and nxt < NEXP:
                            prep_transposes(ei + 1, nxt)
                        for tl in range(W // 128):
                            if e < NEXP:
                                yi = ysc % 3
                                ysc += 1
                            for hf in range(2):
                                yb = YB[yc % 3]
                                yc += 1
                                for hc in range(2):
                                    MM(pb[yb][:], hT[hb][:, hc, tl * 128:(tl + 1) * 128], wd[s_][:, hc, hf * 512:(hf + 1) * 512], hc == 0, hc == 1,
                                       [B_hT[hb][hc], B_wd[s_]], [BP[yb]], inc=(hc == 1))
                                if e < NEXP:
                                    CP(act if evc % 2 == 0 else dve, ysb[yi][:, hf * 512:(hf + 1) * 512], pb[yb][:], [BP[yb]], [B_ysb[yi]])
                                    evc += 1
                                else:
                                    i = blk * 4 + tl
                                    acc = h1[:, i, hf * 512:(hf + 1) * 512]
                                    TT(dve, acc, pb[yb][:], acc, ALU.add, [BP[yb], B_h1[i][hf]], [B_h1[i][hf]])
                            if e < NEXP:
                                act.dma(lambda en, e=e, tl=tl, yi=yi: en.dma_start(out=y_dram[e * CAP + tl * 128:e * CAP + (tl + 1) * 128, :], in_=ysb[yi][:]),
                                        ds_yst[yi], reads=[B_ysb[yi]], writes=[])
                P.barrier()
                w8f = w8_all[:].rearrange("p t k -> p (t k)")
                w8hi = sl[0][:, 0:128]; B_w8hi = Buf("w8hi")
                w8lo = hT[0][:, 0, 0:128]; B_w8lo = Buf("w8lo")
                w8lf = ostg[0][:, 0:128]; B_w8lf = Buf("w8lf")
                CP(dve, w8hi, w8f, B_w8, [B_w8hi])
                TT(dve, w8lf, w8f, w8hi, ALU.subtract, B_w8 + [B_w8hi], [B_w8lf])
                CP(dve, w8lo, w8lf, [B_w8lf], [B_w8lo])
                Dm = [[wt[b_][:].rearrange("p c n -> p (c n)")[:, 0:1024].rearrange("p (k m) -> p k m", m=128) for wt in (wg, wu)] for b_ in range(2)]
                B_Dm = [[Buf(f"Dm{b_}_{j}") for j in range(2)] for b_ in range(2)]
                id3 = identb[:].rearrange("p (o m) -> p o m", o=1).to_broadcast([128, 8, 128])
                gk = 0
                def make_D(i):
                    bf_ = i % 2
                    for j, (wsrc, Bsrc) in enumerate(((w8hi, B_w8hi), (w8lo, B_w8lo))):
                        TT(dve, Dm[bf_][j], id3, wsrc[:, i * 8:(i + 1) * 8].rearrange("p (k o) -> p k o", o=1).to_broadcast([128, 8, 128]), ALU.mult,
                           [B_identb, Bsrc], [B_Dm[bf_][j]])

                make_D(0)
                for i in range(16):
                    bf_ = i % 2
                    for k in range(8):
                        r = gk % NYK
                        gk += 1
                        pool.dma(lambda en, i=i, k=k, r=r: en.indirect_dma_start(
                            out=yk[r][:], out_offset=None, in_=y_dram[:, :], in_offset=bass.IndirectOffsetOnAxis(ap=dest8[:, i * 8 + k:i * 8 + k + 1], axis=0),
                            bounds_check=P.bc_reg(en, NROWS - 1), oob_is_err=False), ds_pull[r], reads=[B_d8[i]], writes=[B_yk[r]])
                        for hf in range(2):
                            bk = 2 * bf_ + hf
                            MM(pb[bk][:], Dm[bf_][0][:, k, :], yk[r][:, hf * 512:(hf + 1) * 512], k == 0, False, [B_Dm[bf_][0], B_yk[r]], [BP[bk]], inc=False)
                            MM(pb[bk][:], Dm[bf_][1][:, k, :], yk[r][:, hf * 512:(hf + 1) * 512], False, k == 7, [B_Dm[bf_][1], B_yk[r]], [BP[bk]], inc=True)
                    if i + 1 < 16:
                        make_D(i + 1)
                    for hf in range(2):
                        acc = h1[:, i, hf * 512:(hf + 1) * 512]
                        TT(dve, acc, pb[2 * bf_ + hf][:], acc, ALU.add, [BP[2 * bf_ + hf], B_h1[i][hf]], [B_h1[i][hf]])
                    o = ostg[i % 2]
                    layer_norm(h1[:, i, :], B_h1[i], o[:], [B_ostg[i % 2]], eng_b=dve, on_act=True)
                    sp.dma(lambda en, i=i, o=o: en.dma_start(out=out[i * 128:(i + 1) * 128, :], in_=o[:]), ds_out[i % 2], reads=[B_ostg[i % 2]], writes=[])
                for d in ds_out:
                    sp.wait_tok(Tok(d.sem, d.count, None))
                P.emit()
    return nc


_CACHE = {}


def _consts(hf):
    identb = np.eye(128, dtype=np.float32).astype(ml_dtypes.bfloat16)
    identf = np.eye(128, dtype=np.float32)
    s = np.arange(64)[:, None]
    t = np.arange(64)[None, :]
    cmask = (t >= s).astype(np.float32)
    rmask = np.ones((128, 512), np.float32)
    rmask[:, ::64] = 0.0
    inv = np.zeros((128, 4, 16), np.float32)
    for g, w in enumerate(WINS):
        if hf == 0:
            inv[:, g, :] = 1.0 / np.minimum(np.arange(1, 17), w).astype(np.float32)
        else:
            inv[:, g, :] = 1.0 / w
    tp = np.arange(128)[:, None]
    tt = np.arange(128)[None, :]
    tri = (tp < tt).astype(np.float32).astype(ml_dtypes.bfloat16)
    iota = np.tile(np.arange(64, dtype=np.float32), (128, 1))
    ecoff = iota * float(CAP)
    return dict(c_identb=identb, c_identf=identf, c_cmask=cmask, c_rmask=rmask, c_invcnt=inv, c_tri=tri, c_iota=iota, c_ecoff=ecoff)


def make_in_maps(inputs, n_cores=8):
    x = np.asarray(inputs["x"], np.float32)
    sq = lambda k: np.ascontiguousarray(np.asarray(inputs[k], np.float32)[0])
    shared = {k: sq(k) for k in ["w_in", "hg_norm_g", "w_pool", "pool_scale", "w_out", "ln1_g", "ln1_b", "w_router", "router_bias",
                                 "w_gate", "w_up", "w_down", "ws_gate", "ws_up", "ws_down", "ln2_g", "ln2_b"]}
    shared["hg_lb_logits"] = np.ascontiguousarray(np.asarray(inputs["hg_lb_logits"], np.float32))
    in_maps = []
    for c in range(n_cores):
        b, hf = c // 2, c % 2
        xl = np.zeros((NLOC, D), np.float32)
        if hf == 1:
            xl[:NMAIN] = x[b, :NMAIN]
        xl[NMAIN:] = x[b, hf * NMAIN:(hf + 1) * NMAIN]
        m = dict(shared)
        m["xloc"] = xl
        m.update(_consts(hf))
        in_maps.append(m)
    return in_maps


def kernel(**inputs):
    if "nc" not in _CACHE:
        _CACHE["nc"] = build()
    nc = _CACHE["nc"]
    in_maps = make_in_maps(inputs)
    res = run_bass_kernel_spmd(nc, in_maps, core_ids=list(range(8)))
    x = inputs["x"]
    outp = np.zeros(x.shape, np.float32)
    for c in range(8):
        b, hf = c // 2, c % 2
        outp[b, hf * NMAIN:(hf + 1) * NMAIN] = res.results[c]["out"]
    return outp
```

```python
import numpy as np
import ml_dtypes
from contextlib import ExitStack
import concourse.bass as bass
import concourse.mybir as mybir
from concourse.bass_utils import run_bass_kernel_spmd

F32 = mybir.dt.float32
BF16 = mybir.dt.bfloat16
AF = mybir.ActivationFunctionType
ALU = mybir.AluOpType
AX = mybir.AxisListType

SEM_ROLL = 6000


class Tok:
    __slots__ = ("sem", "val", "eng")

    def __init__(self, sem, val, eng):
        self.sem, self.val, self.eng = sem, val, eng


class Buf:
    __slots__ = ("name", "w", "r")

    def __init__(self, name):
        self.name, self.w, self.r = name, None, []


class DSem:
    def __init__(self, prog, name):
        self.sem = prog.nc.alloc_semaphore(name)
        self.count = 0
        prog.dsems.append(self)


class Eng:
    def __init__(self, prog, name, is_pe=False, strict=False):
        self.prog, self.name, self.is_pe, self.strict = prog, name, is_pe, strict
        self.items = []
        self.waited = {}
        self.nsem = 0
        self.sem = None
        self.count = 0
        self.pend_r, self.pend_w = [], []
        self._roll()

    def _roll(self):
        self.sem = self.prog.nc.alloc_semaphore(f"c_{self.name}_{self.nsem}")
        self.nsem += 1
        self.count = 0

    def _waits(self, reads, writes):
        deps = []
        for b in reads:
            if b.w is not None:
                deps.append((b.w, True))
        for b in writes:
            if b.w is not None:
                deps.append((b.w, False))
            for t in b.r:
                deps.append((t, False))
        waits = []
        for t, raw in deps:
            if t.eng is self and (self.is_pe or (not raw and not self.strict)):
                continue
            k = id(t.sem)
            if self.waited.get(k, 0) >= t.val:
                continue
            self.waited[k] = t.val
            waits.append((t.sem, t.val))
        return waits

    def op(self, fn, reads=(), writes=(), inc=True):
        reads, writes = list(reads), list(writes)
        waits = self._waits(reads, writes)
        if not inc:
            self.items.append((waits, fn, None, 0))
            self.pend_r.extend(reads)
            self.pend_w.extend(writes)
            return None
        if self.count >= SEM_ROLL:
            self._roll()
        self.count += 1
        tok = Tok(self.sem, self.count, self)
        self.items.append((waits, fn, self.sem, 1))
        for b in reads + self.pend_r:
            b.r.append(tok)
        for b in writes + self.pend_w:
            b.w = tok
            b.r = []
        self.pend_r, self.pend_w = [], []
        return tok

    def dma(self, fn, dsem, reads=(), writes=()):
        reads, writes = list(reads), list(writes)
        waits = self._waits(reads, writes)
        dsem.count += 16
        tok = Tok(dsem.sem, dsem.count, None)
        self.items.append((waits, fn, dsem.sem, 16))
        for b in reads:
            b.r.append(tok)
        for b in writes:
            b.w = tok
            b.r = []
        return tok

    def wait_tok(self, tok):
        k = id(tok.sem)
        if self.waited.get(k, 0) >= tok.val:
            return
        self.waited[k] = tok.val
        self.items.append(([(tok.sem, tok.val)], None, None, 0))

    def replay(self, eng):
        for waits, fn, sem, n in self.items:
            for s, v in waits:
                eng.wait_ge(s, v)
            if fn is None:
                continue
            ins = fn(eng)
            if sem is not None:
                ins.then_inc(sem, n)


class Prog:
    def __init__(self, nc):
        self.nc = nc
        self.dsems = []
        self.pe = Eng(self, "pe", is_pe=True)
        self.act = Eng(self, "act", strict=True)
        self.dve = Eng(self, "dve", strict=True)
        self.pool = Eng(self, "pool", strict=True)
        self.sp = Eng(self, "sp")
        self.engs = [self.pe, self.act, self.dve, self.pool, self.sp]
        self.regcache = {}

    def bc_reg(self, eng, val):
        if val not in self.regcache:
            self.regcache[val] = eng.to_reg(val)
        return self.regcache[val]

    def barrier(self):
        toks = []
        for e in self.engs:
            assert not e.pend_r and not e.pend_w
            if e.count > 0:
                toks.append(Tok(e.sem, e.count, e))
        for d in self.dsems:
            if d.count > 0:
                toks.append(Tok(d.sem, d.count, None))
        for e in self.engs:
            for t in toks:
                if t.eng is e:
                    continue
                e.wait_tok(t)

    def emit(self):
        with self.nc.Block() as block:
            block.tensor(self.pe.replay)
            block.scalar(self.act.replay)
            block.vector(self.dve.replay)
            block.gpsimd(self.pool.replay)
            block.sync(self.sp.replay)
        for e in self.engs:
            e.items = []
        self.regcache = {}


D = 1024
NLOC = 4096
NMAIN = 2048
BLK = 512
NBLK = 8
ALPHA = float(2 ** 0.25)
LN_EPS = 1e-5
RMS_EPS = 1e-6
NEXP = 64
WINS = (2, 4, 8, 16)
CAP = 384
NROWS = NEXP * CAP
I32 = mybir.dt.int32
U32 = mybir.dt.uint32


def build(debug=None, n_exp=NEXP):
    nc = bass.Bass("TRN2", target_bir_lowering=False)

    def din(name, shape, dt=F32):
        return nc.dram_tensor(name, list(shape), dt, kind="ExternalInput").ap()

    def dout(name, shape, dt=F32):
        return nc.dram_tensor(name, list(shape), dt, kind="ExternalOutput").ap()

    xloc = din("xloc", [NLOC, D])
    w_in = din("w_in", [D, 2560])
    lb_logits = din("hg_lb_logits", [2, 512])
    hg_norm_g = din("hg_norm_g", [512])
    w_pool = din("w_pool", [4, 128, 128])
    pool_scale = din("pool_scale", [512])
    w_out = din("w_out", [D, D])
    ln1_g = din("ln1_g", [D]); ln1_b = din("ln1_b", [D])
    w_router = din("w_router", [D, 64])
    router_bias = din("router_bias", [64])
    if debug is None:
        w_gate = din("w_gate", [64, D, 256]); w_up = din("w_up", [64, D, 256]); w_down = din("w_down", [64, 256, D])
        ws_gate = din("ws_gate", [D, 256]); ws_up = din("ws_up", [D, 256]); ws_down = din("ws_down", [256, D])
        ln2_g = din("ln2_g", [D]); ln2_b = din("ln2_b", [D])
    c_identb = din("c_identb", [128, 128], BF16)
    c_identf = din("c_identf", [128, 128])
    c_cmask = din("c_cmask", [64, 64])
    c_rmask = din("c_rmask", [128, 512])
    c_invcnt = din("c_invcnt", [128, 4, 16])
    c_tri = din("c_tri", [128, 128], BF16)
    c_iota = din("c_iota", [128, 64])
    c_ecoff = din("c_ecoff", [128, 64])
    xg_dram = nc.dram_tensor("xg_dram", [NROWS, D], BF16, kind="Internal")
    y_dram = nc.dram_tensor("y_dram", [NROWS, D], BF16, kind="Internal")
    out = dout("out", [NMAIN, D])
    dbg = {}
    if debug == "mixer":
        dbg["mixT"] = dout("dbg_mixT", [128, 8, NMAIN], BF16)
    if debug in ("p4", "p4a", "p4b"):
        dbg["h1"] = dout("dbg_h1", [NMAIN, D])
        dbg["gw"] = dout("dbg_gw", [NMAIN, 64])

    P = Prog(nc)
    pe, act, dve, pool, sp = P.pe, P.act, P.dve, P.pool, P.sp
    glob = ExitStack()

    def sbt(es, name, shape, dt):
        return es.enter_context(nc.sbuf_tensor(name, list(shape), dt))

    def A(out_, in_, func, reads, writes, **kw):
        return act.op(lambda e: e.activation(out=out_, in_=in_, func=func, **kw), reads, writes)

    def TT(E, out_, in0, in1, op, reads, writes):
        return E.op(lambda e: e.tensor_tensor(out=out_, in0=in0, in1=in1, op=op), reads, writes)

    def TS(E, out_, in0, s1, s2, op0, op1, reads, writes, **kw):
        return E.op(lambda e: e.tensor_scalar(out=out_, in0=in0, scalar1=s1, scalar2=s2, op0=op0, op1=op1, **kw), reads, writes)

    def STT(out_, in0, scalar, in1, op0, op1, reads, writes, **kw):
        return dve.op(lambda e: e.scalar_tensor_tensor(out=out_, in0=in0, scalar=scalar, in1=in1, op0=op0, op1=op1, **kw), reads, writes)

    def CP(E, out_, in_, reads, writes):
        if E is act:
            return act.op(lambda e: e.activation(out=out_, in_=in_, func=AF.Copy), reads, writes)
        return E.op(lambda e: e.tensor_copy(out=out_, in_=in_), reads, writes)

    def MM(out_, lhsT, rhs, start, stop, reads, writes, inc):
        return pe.op(lambda e: e.matmul(out_, lhsT=lhsT, rhs=rhs, start=start, stop=stop), reads, writes, inc=inc)

    def TR(out_, in_, ident, reads, writes, inc):
        return pe.op(lambda e: e.transpose(out=out_, in_=in_, identity=ident), reads, writes, inc=inc)

    _bufsem = {}

    def DMA(E, out_, in_, dsem, reads=(), writes=(), **kw):
        reads, writes = list(reads), list(writes)
        key = writes[0] if writes else None
        if key is not None:
            if id(key) not in _bufsem:
                _bufsem[id(key)] = DSem(P, "dq_" + key.name)
            dsem = _bufsem[id(key)]
        return E.dma(lambda e: e.dma_start(out=out_, in_=in_, **kw), dsem, reads, writes)

    with glob:
        pb = [glob.enter_context(nc.psum_tensor(f"pb{i}", [128, 512], F32)) for i in range(8)]
        BP = [Buf(f"pb{i}") for i in range(8)]

        identb = sbt(glob, "identb", [128, 128], BF16); B_identb = Buf("identb")
        onesb = sbt(glob, "onesb", [128, 128], BF16); B_onesb = Buf("onesb")
        cmask = sbt(glob, "cmask", [64, 64], F32); B_cmask = Buf("cmask")
        rmask = sbt(glob, "rmask", [128, 512], F32); B_rmask = Buf("rmask")
        invcnt = sbt(glob, "invcnt", [128, 4, 16], F32); B_invcnt = Buf("invcnt")
        lbl = sbt(glob, "lbl", [128, 2, 4], F32); B_lbl = Buf("lbl")
        lbv = sbt(glob, "lbv", [128, 4], F32); B_lbv = Buf("lbv")
        omlv = sbt(glob, "omlv", [128, 4], F32); B_omlv = Buf("omlv")
        hgn = sbt(glob, "hgn", [128, 4], F32); B_hgn = Buf("hgn")
        pscale = sbt(glob, "pscale", [128, 4], F32); B_pscale = Buf("pscale")
        gw_all = sbt(glob, "gw_all", [128, 16, 64], F32); B_gw = [Buf(f"gw{i}") for i in range(16)]
        w8_all = sbt(glob, "w8_all", [128, 16, 8], F32); B_w8 = [Buf(f"w8_{i}") for i in range(16)]
        dest8 = sbt(glob, "dest8", [128, 128], I32); B_d8 = [Buf(f"d8_{i}") for i in range(16)]
        tri = sbt(glob, "tri", [128, 128], BF16); B_tri = Buf("tri")
        iota64 = sbt(glob, "iota64", [128, 64], F32); B_iota = Buf("iota64")
        ecoff = sbt(glob, "ecoff", [128, 64], F32); B_ecoff = Buf("ecoff")
        B_xg = Buf("xg_dram"); B_yd = Buf("y_dram")
        mixT = sbt(glob, "mixT", [128, 8, NMAIN], BF16)
        B_mix = [[Buf(f"mix{j}_{n}") for n in range(4)] for j in range(8)]

        ds_c = DSem(P, "ds_const")
        DMA(sp, identb[:], c_identb, ds_c, writes=[B_identb])
        DMA(sp, cmask[:], c_cmask, ds_c, writes=[B_cmask])
        DMA(sp, rmask[:], c_rmask, ds_c, writes=[B_rmask])
        DMA(sp, invcnt[:], c_invcnt, ds_c, writes=[B_invcnt])
        DMA(sp, tri[:], c_tri, ds_c, writes=[B_tri])
        DMA(sp, iota64[:], c_iota, ds_c, writes=[B_iota])
        DMA(sp, ecoff[:], c_ecoff, ds_c, writes=[B_ecoff])
        DMA(sp, lbl[:], lb_logits.rearrange("s (h p) -> p s h", p=128), ds_c, writes=[B_lbl], allow_slow_non_contiguous=True)
        DMA(sp, hgn[:], hg_norm_g.rearrange("(h p) -> p h", p=128), ds_c, writes=[B_hgn], allow_slow_non_contiguous=True)
        DMA(sp, pscale[:], pool_scale.rearrange("(h p) -> p h", p=128), ds_c, writes=[B_pscale], allow_slow_non_contiguous=True)
        dve.op(lambda e: e.memset(onesb[:], 1.0), [], [B_onesb])
        zrow = sbt(glob, "zrow", [128, D], BF16); B_zrow = Buf("zrow")
        dve.op(lambda e: e.memset(zrow[:], 0.0), [], [B_zrow])
        ds_zf = DSem(P, "ds_zfill")
        xg_v = xg_dram.ap().rearrange("(p r) d -> p r d", p=128)
        RPP = NROWS // 128
        ZCH = 16

        def zero_fill(j):
            sp.dma(lambda en, j=j: en.dma_start(out=xg_v[:, j * ZCH:(j + 1) * ZCH, :],
                                                in_=zrow[:].rearrange("p (o d) -> p o d", o=1).to_broadcast([128, ZCH, D])),
                   ds_zf, reads=[B_zrow], writes=[])
        NZF = RPP // ZCH
        TT(dve, lbv[:], lbl[:, 0, :], lbl[:, 1, :], ALU.subtract, [B_lbl], [B_lbv])
        A(lbv[:], lbv[:], AF.Sigmoid, [B_lbv], [B_lbv])
        TS(dve, omlv[:], lbv[:], -1.0, 1.0, ALU.mult, ALU.add, [B_lbv], [B_omlv])

        mix = ExitStack()
        with mix:
            w_in_sb = sbt(mix, "w_in_sb", [128, 8, 2560], BF16); B_win = [Buf(f"win{r}") for r in range(5)]
            wpool_sb = sbt(mix, "wpool_sb", [128, 4, 128], BF16); B_wpool = Buf("wpool")
            xb = [sbt(mix, f"xb{j}", [128, D], BF16) for j in range(4)]; B_xb = [Buf(f"xb{j}") for j in range(4)]
            ds_xb = [DSem(P, f"ds_xb{j}") for j in range(4)]
            xTb = sbt(mix, "xTb", [128, 8, BLK], BF16); B_xT = [Buf(f"xT{j}") for j in range(4)]
            vblk = sbt(mix, "vblk", [64, 8, 512], BF16); B_v = [Buf(f"v{c}") for c in range(8)]
            ubuf = sbt(mix, "ubuf", [128, 4, 528], F32); B_u = [Buf(f"u{g}") for g in range(4)]
            pp0 = sbt(mix, "pp0", [128, 528], F32); B_pp0 = Buf("pp0")
            pp1 = sbt(mix, "pp1", [128, 528], F32); B_pp1 = Buf("pp1")
            pfix = sbt(mix, "pfix", [128, 16], F32); B_pfix = Buf("pfix")
            pT = sbt(mix, "pT", [128, 4, 512], BF16); B_pT = [Buf(f"pT{g}") for g in range(4)]
            S_f = sbt(mix, "S_f", [128, 4, 128], F32); B_Sf = [Buf(f"Sf{h}") for h in range(4)]
            S_bf = sbt(mix, "S_bf", [128, 4, 8, 128], BF16); B_Sbf = [[Buf(f"Sbf{h}_{s}") for s in range(8)] for h in range(4)]
            NSET = 4
            names32 = ["sgf", "logf", "b", "eb", "sgT"]
            names16 = ["qt", "kt", "kdT"]
            T = [dict() for _ in range(NSET)]
            BT = [dict() for _ in range(NSET)]
            for s in range(NSET):
                for nm in names32:
                    T[s][nm] = sbt(mix, f"T{s}_{nm}", [128, 512], F32); BT[s][nm] = Buf(f"T{s}_{nm}")
                for nm in names16:
                    T[s][nm] = sbt(mix, f"T{s}_{nm}", [128, 512], BF16); BT[s][nm] = Buf(f"T{s}_{nm}")
                T[s]["kd"] = sbt(mix, f"T{s}_kd", [64, 8, 128], BF16); BT[s]["kd"] = Buf(f"T{s}_kd")
                T[s]["scm"] = sbt(mix, f"T{s}_scm", [64, 8, 64], BF16); BT[s]["scm"] = Buf(f"T{s}_scm")

            ds_win = [DSem(P, f"ds_win{r}") for r in range(5)]

            def load_x(n):
                for j in range(4):
                    r0 = n * BLK + j * 128
                    DMA(pool, xb[j][:], xloc[r0:r0 + 128, :], ds_xb[j], writes=[B_xb[j]])

            load_x(0)
            for r in (1, 2, 0, 3, 4):
                DMA(pool, w_in_sb[:, :, r * 512:(r + 1) * 512],
                    w_in[:, r * 512:(r + 1) * 512].rearrange("(c p) n -> p c n", p=128), ds_win[r], writes=[B_win[r]])
            ds_wp = DSem(P, "ds_wpool")
            DMA(pool, wpool_sb[:], w_pool.rearrange("g c d -> c g d"), ds_wp, writes=[B_wpool])
            dve.op(lambda e: e.memset(S_f[:], 0.0), [], B_Sf)
            dve.op(lambda e: e.memset(S_bf[:], 0.0), [], [b for hb in B_Sbf for b in hb])
            dve.op(lambda e: e.memset(ubuf[:], 0.0), [], B_u)

            TRB = 0
            IPB = [1, 2, 3]
            ipc = [0]

            def next_ip():
                i = IPB[ipc[0] % 3]
                ipc[0] += 1
                return i

            UB = [4, 5]
            SCB = 6
            OB = 7
            trv = pb[TRB][:].bitcast(BF16).rearrange("p (c t) -> p c t", t=128)
            scv = pb[SCB][:64, :].rearrange("p (c t) -> p c t", t=64)

            for n in range(NBLK):
                is_main = n >= 4
                mb = n - 4
                for j in range(4):
                    for c in range(8):
                        TR(trv[:, c, :], xb[j][:, c * 128:(c + 1) * 128], identb[:], [B_xb[j], B_identb], [BP[TRB]], inc=(c == 7))
                    xt_tok = CP(dve if j % 2 == 0 else act, xTb[:, :, j * 128:(j + 1) * 128], trv, [BP[TRB]], [B_xT[j]])
                if n + 1 < NBLK:
                    load_x(n + 1)
                if n >= 2:
                    sp.wait_tok(xt_tok)
                    for j in range((n - 2) * NZF // (NBLK - 2), (n - 1) * NZF // (NBLK - 2)):
                        zero_fill(j)
                for c in range(8):
                    bi = next_ip()
                    for kc in range(8):
                        MM(pb[bi][:64, :], xTb[:, kc, c * 64:(c + 1) * 64], w_in_sb[:, kc, 1024:1536], kc == 0, kc == 7,
                           [B_xT[c // 2], B_win[2]], [BP[bi]], inc=(kc == 7))
                    CP(act, vblk[:, c, :], pb[bi][:64, :], [BP[bi]], [B_v[c]])
                if is_main or n == 3:
                    for g in range(4):
                        bi = next_ip()
                        for kc in range(8):
                            MM(pb[bi][:], w_in_sb[:, kc, 2048 + g * 128:2048 + (g + 1) * 128], xTb[:, kc, :], kc == 0, kc == 7,
                               B_xT + [B_win[4]], [BP[bi]], inc=(kc == 7))
                        CP(dve, ubuf[:, g, 16:528], pb[bi][:], [BP[bi]], [B_u[g]])
                HS = range(4)
                fb, qb, gb = {}, {}, {}
                for h in HS:
                    bi = next_ip(); fb[h] = bi
                    for kc in range(8):
                        MM(pb[bi][:], w_in_sb[:, kc, 512 + h * 128:512 + (h + 1) * 128], xTb[:, kc, :], kc == 0, kc == 7,
                           B_xT + [B_win[1]], [BP[bi]], inc=(kc == 7))
                    A(T[h]["sgf"][:], pb[bi][:], AF.Sigmoid, [BP[bi]], [BT[h]["sgf"]])
                for h in HS:
                    t, bt = T[h], BT[h]
                    TS(dve, t["sgf"][:], t["sgf"][:], omlv[:, h:h + 1], lbv[:, h:h + 1], ALU.mult, ALU.add,
                       [bt["sgf"], B_omlv, B_lbv], [bt["sgf"]])
                for h in HS:
                    t, bt = T[h], BT[h]
                    A(t["logf"][:], t["sgf"][:], AF.Ln, [bt["sgf"]], [bt["logf"]])
                for h in HS:
                    t, bt = T[h], BT[h]
                    dve.op(lambda e, t=t: e.tensor_tensor_scan(out=t["b"][:], data0=rmask[:], data1=t["logf"][:], initial=0.0,
                                                                op0=ALU.mult, op1=ALU.add),
                           [bt["logf"], B_rmask], [bt["b"]])
                    TS(pool, t["sgf"][:], t["sgf"][:], -1.0, 1.0, ALU.mult, ALU.add, [bt["sgf"]], [bt["sgf"]])
                for h in HS:
                    t, bt = T[h], BT[h]
                    A(t["eb"][:], t["b"][:], AF.Exp, [bt["b"]], [bt["eb"]])
                    A(t["logf"][:], t["b"][:], AF.Exp, [bt["b"]], [bt["logf"]], scale=-1.0)
                for h in HS:
                    t, bt = T[h], BT[h]
                    TT(dve, t["kt"][:], t["sgf"][:], t["logf"][:], ALU.mult, [bt["sgf"], bt["logf"]], [bt["kt"]])
                    eb3 = t["eb"][:].rearrange("p (c j) -> p c j", j=64)
                    TT(dve, t["kdT"][:].rearrange("p (c j) -> p c j", j=64), t["kt"][:].rearrange("p (c j) -> p c j", j=64),
                       eb3[:, :, 63:64].to_broadcast([128, 8, 64]), ALU.mult, [bt["kt"], bt["eb"]], [bt["kdT"]])
                if is_main:
                    for h in HS:
                        bi = next_ip()
                        for kc in range(8):
                            MM(pb[bi][:], w_in_sb[:, kc, h * 128:(h + 1) * 128], xTb[:, kc, :], kc == 0, kc == 7,
                               B_xT + [B_win[0]], [BP[bi]], inc=(kc == 7))
                        A(T[h]["b"][:], pb[bi][:], AF.Silu, [BP[bi]], [BT[h]["b"]])
                    for h in HS:
                        bi = next_ip()
                        for kc in range(8):
                            MM(pb[bi][:], w_in_sb[:, kc, 1536 + h * 128:1536 + (h + 1) * 128], xTb[:, kc, :], kc == 0, kc == 7,
                               B_xT + [B_win[3]], [BP[bi]], inc=(kc == 7))
                        A(T[h]["sgT"][:], pb[bi][:], AF.Silu, [BP[bi]], [BT[h]["sgT"]])
                    for h in HS:
                        t, bt = T[h], BT[h]
                        TT(dve, t["qt"][:], t["b"][:], t["eb"][:], ALU.mult, [bt["b"], bt["eb"]], [bt["qt"]])
                for h in HS:
                    t, bt = T[h], BT[h]
                    for c in range(8):
                        TR(trv[:64, c, :], t["kdT"][:, c * 64:(c + 1) * 64], identb[:], [bt["kdT"], B_identb], [BP[TRB]], inc=(c == 7))
                    CP(act, t["kd"][:], trv[:64, :, :], [BP[TRB]], [bt["kd"]])
                if is_main:
                    for h in HS:
                        t, bt = T[h], BT[h]
                        for c in range(8):
                            MM(scv[:, c, :], t["kt"][:, c * 64:(c + 1) * 64], t["qt"][:, c * 64:(c + 1) * 64], True, True,
                               [bt["kt"], bt["qt"]], [BP[SCB]], inc=(c == 7))
                        TT(dve, t["scm"][:], scv, cmask[:].rearrange("p (o t) -> p o t", o=1).to_broadcast([64, 8, 64]), ALU.mult,
                           [BP[SCB], B_cmask], [bt["scm"]])
                if is_main:
                    for g in range(4):
                        w = WINS[g]
                        src, Bsrc = ubuf[:, g, :], B_u[g]
                        d = 1
                        lvl = 0
                        while d < w:
                            dst, Bdst = (pp0, B_pp0) if lvl % 2 == 0 else (pp1, B_pp1)
                            lo = 2 * d - 1
                            TT(pool, dst[:, lo:528], src[:, lo:528], src[:, lo - d:528 - d], ALU.add, [Bsrc], [Bdst])
                            src, Bsrc = dst[:], Bdst
                            d *= 2
                            lvl += 1
                        STT(pT[:, g, :], src[:, 16:528], 1.0 / w, ubuf[:, g, 16:528], ALU.mult, ALU.subtract, [Bsrc, B_u[g]], [B_pT[g]])
                        if mb == 0:
                            TT(dve, pfix[:], src[:, 16:32], invcnt[:, g, :], ALU.mult, [Bsrc, B_invcnt], [B_pfix])
                            TT(dve, pT[:, g, 0:16], pfix[:], ubuf[:, g, 16:32], ALU.subtract, [B_pfix, B_u[g]], [B_pT[g]])
                        bi = next_ip()
                        MM(pb[bi][:], wpool_sb[:, g, :], pT[:, g, :], True, True, [B_wpool, B_pT[g]], [BP[bi]], inc=True)
                        A(mixT[:, 4 + g, mb * BLK:(mb + 1) * BLK], pb[bi][:], AF.Copy, [BP[bi], B_pscale], [B_mix[4 + g][mb]], scale=pscale[:, g:g + 1])
                OBK = [OB] + IPB

                def emit_U(c):
                    ub = UB[c % 2]
                    for h in HS:
                        MM(pb[ub][:, h * 128:(h + 1) * 128], T[h]["kd"][:, c, :], vblk[:, c, h * 128:(h + 1) * 128], True, True,
                           [BT[h]["kd"], B_v[c]], [BP[ub]], inc=(h == 3))

                emit_U(0)
                for c in range(8):
                    if c + 1 < 8:
                        emit_U(c + 1)
                    if is_main:
                        for h in HS:
                            t, bt = T[h], BT[h]
                            MM(pb[OBK[h]][:, c * 64:(c + 1) * 64], S_bf[:, h, c, :], t["qt"][:, c * 64:(c + 1) * 64], True, False,
                               [B_Sbf[h][c], bt["qt"]], [BP[OBK[h]]], inc=False)
                            MM(pb[OBK[h]][:, c * 64:(c + 1) * 64], vblk[:, c, h * 128:(h + 1) * 128], t["scm"][:, c, :], False, True,
                               [B_v[c], bt["scm"]], [BP[OBK[h]]], inc=True)
                    ub = UB[c % 2]
                    for h in HS:
                        t, bt = T[h], BT[h]
                        STT(S_f[:, h, :], S_f[:, h, :], t["eb"][:, c * 64 + 63:c * 64 + 64], pb[ub][:, h * 128:(h + 1) * 128],
                            ALU.mult, ALU.add, [B_Sf[h], bt["eb"], BP[ub]], [B_Sf[h]])
                    nslot = (c + 1) % 8
                    if is_main or (n == 3 and c == 7):
                        for h in HS:
                            CP(act, S_bf[:, h, nslot, :], S_f[:, h, :], [B_Sf[h]], [B_Sbf[h][nslot]])
                if is_main:
                    for h in HS:
                        A(T[h]["kdT"][:], pb[OBK[h]][:], AF.Square, [BP[OBK[h]]], [BT[h]["kdT"]])
                    for h in HS:
                        bs = UB[h % 2]
                        MM(pb[bs][:], onesb[:], T[h]["kdT"][:], True, True, [B_onesb, BT[h]["kdT"]], [BP[bs]], inc=True)
                        A(T[h]["logf"][:], pb[bs][:], AF.Ln, [BP[bs]], [BT[h]["logf"]], scale=1.0 / 128.0, bias=RMS_EPS)
                    for h in HS:
                        A(T[h]["logf"][:], T[h]["logf"][:], AF.Exp, [BT[h]["logf"]], [BT[h]["logf"]], scale=-0.5)
                    for h in HS:
                        t, bt = T[h], BT[h]
                        TT(dve, t["sgf"][:], pb[OBK[h]][:], t["logf"][:], ALU.mult, [BP[OBK[h]], bt["logf"]], [bt["sgf"]])
                        STT(mixT[:, h, mb * BLK:(mb + 1) * BLK], t["sgf"][:], hgn[:, h:h + 1], t["sgT"][:], ALU.mult, ALU.mult,
                            [bt["sgf"], B_hgn, bt["sgT"]], [B_mix[h][mb]])
                if is_main or n == 3:
                    for g in range(4):
                        CP(pool, ubuf[:, g, 0:16], ubuf[:, g, 512:528], [B_u[g]], [B_u[g]])

            if debug == "mixer":
                ds_dbg = DSem(P, "ds_dbg")
                tk = DMA(sp, dbg["mixT"], mixT[:], ds_dbg, reads=[b for row in B_mix for b in row])
                sp.wait_tok(tk)
                P.emit()
                return nc
            P.barrier()
            P.emit()

        post = ExitStack()
        with post:
            h1 = sbt(post, "h1", [128, 16, D], F32); B_h1 = [[Buf(f"h1_{i}_{hf}") for hf in range(2)] for i in range(16)]
            h1T = sbt(post, "h1T", [128, 8, NMAIN], BF16); B_h1T = [Buf(f"h1T{i}") for i in range(16)]
            lng = sbt(post, "lng", [128, D], F32); B_lng = Buf("lng")
            lnb = sbt(post, "lnb", [128, D], F32); B_lnb = Buf("lnb")
            ds_ln = DSem(P, "ds_ln")
            DMA(sp, lng[:], ln1_g.partition_broadcast(128), ds_ln, writes=[B_lng])
            DMA(sp, lnb[:], ln1_b.partition_broadcast(128), ds_ln, writes=[B_lnb])
            stats = sbt(post, "stats", [128, 2, 6], F32); B_stats = Buf("stats")
            mv = sbt(post, "mv", [128, 2], F32); B_mv = Buf("mv")
            rstd = sbt(post, "rstd", [128, 1], F32); B_rstd = Buf("rstd")

            nmr = sbt(post, "nmr", [128, 1], F32); B_nmr = Buf("nmr")

            def layer_norm(src, Bsrc_list, dst, Bdst_list, eng_b=pool, on_act=False):
                for hf in range(2):
                    dve.op(lambda e, hf=hf: e.bn_stats(out=stats[:, hf, :], in_=src[:, hf * 512:(hf + 1) * 512]), Bsrc_list, [B_stats])
                dve.op(lambda e: e.bn_aggr(out=mv[:], in_=stats[:].rearrange("p a b -> p (a b)")), [B_stats], [B_mv])
                A(rstd[:], mv[:, 1:2], AF.Ln, [B_mv], [B_rstd], bias=LN_EPS)
                A(rstd[:], rstd[:], AF.Exp, [B_rstd], [B_rstd], scale=-0.5)
                if on_act:
                    TS(dve, nmr[:], mv[:, 0:1], rstd[:, 0:1], -1.0, ALU.mult, ALU.mult, [B_mv, B_rstd], [B_nmr])
                    A(dst, src, AF.Identity, Bsrc_list + [B_rstd, B_nmr], Bdst_list, scale=rstd[:, 0:1], bias=nmr[:, 0:1])
                else:
                    TS(dve, dst, src, mv[:, 0:1], rstd[:, 0:1], ALU.subtract, ALU.mult, Bsrc_list + [B_mv, B_rstd], Bdst_list)
                TT(dve, dst, dst, lng[:], ALU.mult, Bdst_list + [B_lng], Bdst_list)
                TT(eng_b, dst, dst, lnb[:], ALU.add, Bdst_list + [B_lnb], Bdst_list)

            p4 = ExitStack()
            with p4:
                wout_sb = sbt(p4, "wout_sb", [128, 8, D], BF16); B_wout = Buf("wout")
                wr_sb = sbt(p4, "wr_sb", [128, 8, 64], F32); B_wr = Buf("wr")
                rbias = sbt(p4, "rbias", [128, 64], F32); B_rbias = Buf("rbias")
                xres = [sbt(p4, f"xres{i}", [128, D], F32) for i in range(2)]; B_xres = [Buf(f"xres{i}") for i in range(2)]
                ds_xres = [DSem(P, f"ds_xres{i}") for i in range(2)]
                rres = sbt(p4, "rres", [128, D], F32); B_rres = Buf("rres")
                hhi2 = [sbt(p4, f"hhi{q}", [128, D], BF16) for q in range(2)]; B_hhi2 = [Buf(f"hhi{q}") for q in range(2)]
                maskb = sbt(p4, "maskb", [128, 16, 64], BF16); B_mask = [Buf(f"mask{i}") for i in range(16)]
                hlo = sbt(p4, "hlo", [128, D], BF16); B_hlo = Buf("hlo")
                loT = sbt(p4, "loT", [128, 8, 128], BF16); B_loT = Buf("loT")
                whi = sbt(p4, "whi", [128, 8, 64], BF16); B_whi = Buf("whi")
                wlo = sbt(p4, "wlo", [128, 8, 64], BF16); B_wlo = Buf("wlo")
                sc_all = sbt(p4, "sc_all", [128, 16, 64], F32); B_sc = [Buf(f"sc{i}") for i in range(16)]
                cumb = sbt(p4, "cumb", [128, 17, 64], BF16); B_cum = [Buf(f"cum{i}") for i in range(17)]
                hhiB = [sbt(p4, f"hhiB{q}", [128, D], BF16) for q in range(2)]; B_hhiB = [Buf(f"hhiB{q}") for q in range(2)]
                G = 4
                rt = {}
                Brt = {}
                for nm, shp in [("sel", [128, G * 64]), ("eq", [128, G * 64]), ("sel2", [128, G * 64]), ("selm", [128, G * 64]), ("w", [128, G * 64]),
                                ("dall", [128, G * 64]),
                                ("m1", [128, G * 8]), ("m2", [128, G * 8]), ("gs", [128, G * 8]), ("g8", [128, G * 8]), ("pen", [128, G * 8]),
                                ("t8", [128, G * 8]), ("i8f", [128, G * 8]), ("d8f", [128, G * 8]), ("okm", [128, G * 8]),
                                ("wsum", [128, G]), ("rs", [128, G])]:
                    rt[nm] = sbt(p4, f"rt_{nm}", shp, F32); Brt[nm] = Buf(f"rt_{nm}")
                for new_nm, old_nm in (("oh", "eq"), ("junk", "sel2"), ("ovf", "selm")):
                    rt[new_nm], Brt[new_nm] = rt[old_nm], Brt[old_nm]
                rt_i8 = sbt(p4, "rt_i8", [128, G * 8], U32); B_i8 = Buf("rt_i8")
                ds_w4 = DSem(P, "ds_w4")
                DMA(pool, wout_sb[:], w_out.rearrange("(c p) n -> p c n", p=128), ds_w4, writes=[B_wout])
                DMA(sp, wr_sb[:], w_router.rearrange("(c p) n -> p c n", p=128), ds_ln, writes=[B_wr])
                DMA(sp, rbias[:], router_bias.partition_broadcast(128), ds_ln, writes=[B_rbias])
                CP(dve, whi[:], wr_sb[:], [B_wr], [B_whi])
                TT(dve, wlo[:], wr_sb[:], whi[:], ALU.subtract, [B_wr, B_whi], [B_wlo])
                dve.op(lambda e: e.memset(cumb[:, 0, :], 0.0), [], [B_cum[0]])

                ds_push = [DSem(P, f"ds_push{k}") for k in range(16)]

                def load_xres(i):
                    DMA(sp, xres[i % 2][:], xloc[NMAIN + i * 128:NMAIN + (i + 1) * 128, :], ds_xres[i % 2], writes=[B_xres[i % 2]])

                def bc3(ap2, n_mid, n_in):
                    return ap2.rearrange("p (g o) -> p g o", o=1).to_broadcast([128, n_mid, n_in])

                def stage_a1(i):
                    blk = i // 4
                    for hf in range(2):
                        for j in range(8):
                            MM(pb[hf][:], mixT[:, j, i * 128:(i + 1) * 128], wout_sb[:, j, hf * 512:(hf + 1) * 512], j == 0, j == 7,
                               [B_mix[j][blk], B_wout], [BP[hf]], inc=(j == 7))
                        STT(rres[:, hf * 512:(hf + 1) * 512], xres[i % 2][:, hf * 512:(hf + 1) * 512], ALPHA, pb[hf][:], ALU.mult, ALU.add,
                            [B_xres[i % 2], BP[hf]], [B_rres])
                    layer_norm(rres[:], [B_rres], h1[:, i, :], B_h1[i], eng_b=dve, on_act=True)

                def stage_a2(i):
                    hhi, B_hhi = hhi2[i % 2], B_hhi2[i % 2]
                    CP(act, hhi[:], h1[:, i, :], B_h1[i], [B_hhi])
                    TT(dve, hlo[:], h1[:, i, :], hhi[:], ALU.subtract, B_h1[i] + [B_hhi], [B_hlo])
                    tv2 = pb[2][:].bitcast(BF16).rearrange("p (c t) -> p c t", t=128)
                    tv3 = pb[3][:].bitcast(BF16).rearrange("p (c t) -> p c t", t=128)
                    for c in range(8):
                        TR(tv2[:, c, :], hhi[:, c * 128:(c + 1) * 128], identb[:], [B_hhi, B_identb], [BP[2]], inc=(c == 7))
                    for c in range(8):
                        TR(tv3[:, c, :], hlo[:, c * 128:(c + 1) * 128], identb[:], [B_hlo, B_identb], [BP[3]], inc=(c == 7))
                    CP(act, h1T[:, :, i * 128:(i + 1) * 128], tv2, [BP[2]], [B_h1T[i]])
                    CP(act, loT[:], tv3, [BP[3]], [B_loT])
                    nmm = 0
                    for (lt, Bl, rw, Br) in ((h1T, B_h1T[i], whi, B_whi), (None, B_loT, whi, B_whi), (h1T, B_h1T[i], wlo, B_wlo)):
                        for c in range(8):
                            lhs = loT[:, c, :] if lt is None else h1T[:, c, i * 128:(i + 1) * 128]
                            MM(pb[4][:, 0:64], lhs, rw[:, c, :], nmm == 0, nmm == 23, [Bl, Br], [BP[4]], inc=(nmm == 23))
                            nmm += 1
                    A(sc_all[:, i, :], pb[4][:, 0:64], AF.Exp, [BP[4]], [B_sc[i]], scale=-1.0)
                    TS(dve, sc_all[:, i, :], sc_all[:, i, :], 1.0, None, ALU.add, ALU.bypass, [B_sc[i]], [B_sc[i]])
                    dve.op(lambda e, i=i: e.reciprocal(out=sc_all[:, i, :], in_=sc_all[:, i, :]), [B_sc[i]], [B_sc[i]])

                def stage_b(g):
                    tl = list(range(G * g, G * g + G))
                    Bsel = [Brt["sel"]]
                    Bsc = [B_sc[i] for i in tl]
                    Bgw = [B_gw[i] for i in tl]
                    sc2d = sc_all[:, G * g:G * g + G, :].rearrange("p t e -> p (t e)")
                    sel2d = rt["sel"][:]
                    sel44 = sel2d.rearrange("p (q e) -> p q e", e=8)
                    TT(dve, sel2d.rearrange("p (t e) -> p t e", e=64), sc_all[:, G * g:G * g + G, :],
                       rbias[:].rearrange("p (o e) -> p o e", o=1).to_broadcast([128, G, 64]), ALU.add, Bsc + [B_rbias], Bsel)
                    gw2d = gw_all[:, G * g:G * g + G, :].rearrange("p t e -> p (t e)")
                    NQ = G * 8
                    dve.op(lambda e: e.tensor_reduce(out=rt["m1"][:], in_=sel44, axis=AX.X, op=ALU.max), Bsel, [Brt["m1"]])
                    TT(dve, rt["eq"][:].rearrange("p (q e) -> p q e", e=8), sel44, bc3(rt["m1"][:], NQ, 8), ALU.is_equal, Bsel + [Brt["m1"]], [Brt["eq"]])
                    STT(rt["sel2"][:], rt["eq"][:], -1.0e9, sel2d, ALU.mult, ALU.add, [Brt["eq"]] + Bsel, [Brt["sel2"]])
                    dve.op(lambda e: e.tensor_reduce(out=rt["m2"][:], in_=rt["sel2"][:].rearrange("p (q e) -> p q e", e=8), axis=AX.X, op=ALU.max),
                           [Brt["sel2"]], [Brt["m2"]])
                    TT(dve, rt["gs"][:], rt["m1"][:], rt["m2"][:], ALU.add, [Brt["m1"], Brt["m2"]], [Brt["gs"]])
                    for t in range(G):
                        dve.op(lambda e, t=t: e.max(out=rt["g8"][:, t * 8:(t + 1) * 8], in_=rt["gs"][:, t * 8:(t + 1) * 8]), [Brt["gs"]], [Brt["g8"]])
                    g83 = rt["g8"][:].rearrange("p (t k) -> p t k", k=8)
                    TT(dve, rt["pen"][:].rearrange("p (t k) -> p t k", k=8), rt["gs"][:].rearrange("p (t k) -> p t k", k=8),
                       g83[:, :, 3:4].to_broadcast([128, G, 8]), ALU.is_lt, [Brt["gs"], Brt["g8"]], [Brt["pen"]])
                    TS(dve, rt["pen"][:], rt["pen"][:], -1.0e9, None, ALU.mult, ALU.bypass, [Brt["pen"]], [Brt["pen"]])
                    TT(dve, rt["selm"][:].rearrange("p (q e) -> p q e", e=8), sel44, bc3(rt["pen"][:], NQ, 8), ALU.add, Bsel + [Brt["pen"]], [Brt["selm"]])
                    for t in range(G):
                        dve.op(lambda e, t=t: e.max(out=rt["t8"][:, t * 8:(t + 1) * 8], in_=rt["selm"][:, t * 64:(t + 1) * 64]), [Brt["selm"]], [Brt["t8"]])
                    t83 = rt["t8"][:].rearrange("p (t k) -> p t k", k=8)
                    selm3 = rt["selm"][:].rearrange("p (t e) -> p t e", e=64)
                    w3 = rt["w"][:].rearrange("p (t e) -> p t e", e=64)
                    TT(dve, w3, selm3, t83[:, :, 7:8].to_broadcast([128, G, 64]), ALU.is_ge, [Brt["selm"], Brt["t8"]], [Brt["w"]])
                    TT(dve, rt["w"][:], rt["w"][:], sc2d, ALU.mult, [Brt["w"]] + Bsc, [Brt["w"]])
                    dve.op(lambda e: e.tensor_reduce(out=rt["wsum"][:], in_=w3, axis=AX.X, op=ALU.add), [Brt["w"]], [Brt["wsum"]])
                    dve.op(lambda e: e.reciprocal(out=rt["rs"][:], in_=rt["wsum"][:]), [Brt["wsum"]], [Brt["rs"]])
                    STT(gw_all[:, G * g:G * g + G, :], w3, 2.5, bc3(rt["rs"][:], G, 64), ALU.mult, ALU.mult, [Brt["w"], Brt["rs"]], Bgw)
                    if debug is not None:
                        return
                    yield
                    for t, i in enumerate(tl):
                        TS(dve, maskb[:, i, :], gw_all[:, i, :], 0.0, None, ALU.is_gt, ALU.bypass, [B_gw[i]], [B_mask[i]])
                        TT(dve, cumb[:, i + 1, :], cumb[:, i, :], maskb[:, i, :], ALU.add, [B_cum[i], B_mask[i]], [B_cum[i + 1]])
                        MM(pb[5][:, t * 64:(t + 1) * 64], tri[:], maskb[:, i, :], True, False, [B_tri, B_mask[i]], [BP[5]], inc=False)
                        MM(pb[5][:, t * 64:(t + 1) * 64], onesb[:], cumb[:, i, :], False, True, [B_onesb, B_cum[i]], [BP[5]], inc=(t == G - 1))
                    ec3 = ecoff[:].rearrange("p (o e) -> p o e", o=1).to_broadcast([128, G, 64])
                    io3 = iota64[:].rearrange("p (o e) -> p o e", o=1).to_broadcast([128, G, 64])
                    pos3 = pb[5][:, 0:G * 64].rearrange("p (t e) -> p t e", e=64)
                    dall3 = rt["dall"][:].rearrange("p (t e) -> p t e", e=64)
                    TT(dve, dall3, pos3, ec3, ALU.add, [BP[5], B_ecoff], [Brt["dall"]])
                    TS(dve, rt["ovf"][:], pb[5][:, 0:G * 64], float(CAP), 1.0e6, ALU.is_ge, ALU.mult, [BP[5]], [Brt["ovf"]])
                    TT(dve, rt["dall"][:], rt["dall"][:], rt["ovf"][:], ALU.add, [Brt["dall"], Brt["ovf"]], [Brt["dall"]])
                    yield
                    for t, i in enumerate(tl):
                        dve.op(lambda e, i=i: e.max(out=w8_all[:, i, :], in_=gw_all[:, i, :]), [B_gw[i]], [B_w8[i]])
                        dve.op(lambda e, i=i, t=t: e.max_index(out=rt_i8[:, t * 8:(t + 1) * 8], in_max=w8_all[:, i, :], in_values=gw_all[:, i, :]),
                               [B_gw[i], B_w8[i]], [B_i8])
                    CP(dve, rt["i8f"][:], rt_i8[:], [B_i8], [Brt["i8f"]])
                    i83 = rt["i8f"][:].rearrange("p (t k) -> p t k", k=8)
                    d83 = rt["d8f"][:].rearrange("p (t k) -> p t k", k=8)
                    oh3 = rt["oh"][:].rearrange("p (t e) -> p t e", e=64)
                    jk3 = rt["junk"][:].rearrange("p (t e) -> p t e", e=64)
                    for k in range(8):
                        TT(dve, oh3, io3, i83[:, :, k:k + 1].to_broadcast([128, G, 64]), ALU.is_equal, [B_iota, Brt["i8f"]], [Brt["oh"]])
                        TT(dve, rt["junk"][:], rt["oh"][:], rt["dall"][:], ALU.mult, [Brt["oh"], Brt["dall"]], [Brt["junk"]])
                        dve.op(lambda e, k=k: e.tensor_reduce(out=d83[:, :, k], in_=jk3, axis=AX.X, op=ALU.add), [Brt["junk"]], [Brt["d8f"]])
                    CP(dve, dest8[:, G * g * 8:(G * g + G) * 8], rt["d8f"][:], [Brt["d8f"]], [B_d8[i] for i in tl])
                    TS(dve, rt["okm"][:], rt["d8f"][:], float(NROWS), None, ALU.is_lt, ALU.bypass, [Brt["d8f"]], [Brt["okm"]])
                    w8g = w8_all[:, G * g:G * g + G, :].rearrange("p t k -> p (t k)")
                    TT(dve, w8g, w8g, rt["okm"][:], ALU.mult, [B_w8[i] for i in tl] + [Brt["okm"]], [B_w8[i] for i in tl])
                    yield
                    for t, i in enumerate(tl):
                        hb_, Bhb = hhiB[i % 2], B_hhiB[i % 2]
                        CP(act, hb_[:].rearrange("t (c p) -> t c p", p=128), h1[:, i, :].rearrange("t (p c) -> t c p", c=8), B_h1[i], [Bhb])
                        for k in range(8):
                            pool.dma(lambda e, i=i, k=k, hb_=hb_: e.indirect_dma_start(
                                out=xg_dram[:, :], out_offset=bass.IndirectOffsetOnAxis(ap=dest8[:, i * 8 + k:i * 8 + k + 1], axis=0), in_=hb_[:], in_offset=None,
                                bounds_check=P.bc_reg(e, NROWS - 1), oob_is_err=False), ds_push[(i % 2) * 8 + k], reads=[Bhb, B_d8[i]], writes=[])

                def drain(gen):
                    for _ in gen:
                        pass

                load_xres(0)
                stage_a1(0)
                pending = None
                for i in range(16):
                    if i + 1 < 16:
                        load_xres(i + 1)
                        stage_a1(i + 1)
                    if debug == "p4a":
                        continue
                    stage_a2(i)
                    if pending is not None:
                        if next(pending, "done") == "done":
                            pending = None
                    if i % G == G - 1:
                        if pending is not None:
                            drain(pending)
                        pending = stage_b(i // G)
                if pending is not None:
                    drain(pending)

                if debug in ("p4", "p4a", "p4b"):
                    ds_dbg = DSem(P, "ds_dbg")
                    tk = DMA(sp, dbg["h1"].rearrange("(i p) d -> p i d", p=128), h1[:], ds_dbg, reads=[b for r in B_h1 for b in r])
                    if debug != "p4a":
                        tk = DMA(sp, dbg["gw"].rearrange("(i p) e -> p i e", p=128), gw_all[:], ds_dbg, reads=B_gw)
                    sp.wait_tok(tk)
                    P.emit()
                    return nc
                P.barrier()
                P.emit()

            moe = ExitStack()
            with moe:
                NWB = 2
                wg = [sbt(moe, f"wg{i}", [128, 8, 256], BF16) for i in range(NWB)]
                wu = [sbt(moe, f"wu{i}", [128, 8, 256], BF16) for i in range(NWB)]
                wd = [sbt(moe, f"wd{i}", [128, 2, D], BF16) for i in range(NWB)]
                B_wg = [Buf(f"wg{i}") for i in range(NWB)]; B_wu = [Buf(f"wu{i}") for i in range(NWB)]; B_wd = [Buf(f"wd{i}") for i in range(NWB)]
                xs = [sbt(moe, f"xs{i}", [128, D], BF16) for i in range(4)]; B_xs = [Buf(f"xs{i}") for i in range(4)]
                flat = mixT[:].rearrange("p a b -> p (a b)")
                xgT = [flat[:, i * 8 * CAP:(i + 1) * 8 * CAP].rearrange("p (c t) -> p c t", t=CAP) for i in range(2)]
                B_xgT = [[Buf(f"xgT{i}_{q}") for q in range(CAP // 128)] for i in range(2)]
                sl = [sbt(moe, f"sl{i}", [128, 512], BF16) for i in range(2)]; B_sl = [Buf(f"sl{i}") for i in range(2)]
                hT = [sbt(moe, f"hT{i}", [128, 2, 512], BF16) for i in range(2)]; B_hT = [[Buf(f"hT{i}_{hc}") for hc in range(2)] for i in range(2)]
                ysb = [flat[:, 12288 + i * D:12288 + (i + 1) * D] for i in range(3)]; B_ysb = [Buf(f"ysb{i}") for i in range(3)]
                NYK = 8
                yk = [flat[:, 8192 + i * D:8192 + (i + 1) * D] for i in range(4)] + [sbt(moe, f"yk{i}", [128, D], BF16) for i in range(4, NYK)]
                B_yk = [Buf(f"yk{i}") for i in range(NYK)]
                ostg = [sbt(moe, f"ostg{i}", [128, D], F32) for i in range(2)]; B_ostg = [Buf(f"ostg{i}") for i in range(2)]
                ds_out = [DSem(P, f"ds_out{i}") for i in range(2)]
                ds_yst = [DSem(P, f"ds_yst{i}") for i in range(3)]
                ds_pull = [DSem(P, f"ds_pull{i}") for i in range(8)]
                DMA(sp, lng[:], ln2_g.partition_broadcast(128), None, writes=[B_lng])
                DMA(sp, lnb[:], ln2_b.partition_broadcast(128), None, writes=[B_lnb])
                for q in range(NYK):
                    pool.op(lambda e, q=q: e.memset(yk[q][:], 0.0), [], [B_yk[q]])

                def load_expert(e):
                    s_ = e % NWB
                    if e < NEXP:
                        g_ap, u_ap, d_ap = w_gate[e], w_up[e], w_down[e]
                    else:
                        g_ap, u_ap, d_ap = ws_gate, ws_up, ws_down
                    lay = "(p c) n -> p c n" if e < NEXP else "(c p) n -> p c n"
                    DMA(pool, wg[s_][:], g_ap.rearrange(lay, p=128), None, writes=[B_wg[s_]])
                    DMA(pool, wu[s_][:], u_ap.rearrange(lay, p=128), None, writes=[B_wu[s_]])
                    DMA(pool, wd[s_][:], d_ap.rearrange("(c p) n -> p c n", p=128), None, writes=[B_wd[s_]])

                experts = list(range(n_exp)) + [NEXP]
                load_expert(experts[0])
                for i in range(16):
                    for hf in range(2):
                        A(h1[:, i, hf * 512:(hf + 1) * 512], h1[:, i, hf * 512:(hf + 1) * 512], AF.Copy, [B_h1[i][hf]], [B_h1[i][hf]], scale=ALPHA)
                TRBS = [0, 1]
                trvs = [pb[b_][:].bitcast(BF16).rearrange("p (c t) -> p c t", t=128) for b_ in TRBS]
                GUB = [2, 3, 4]
                guc = [0]
                YB = [5, 6, 7]
                yc = 0
                uidx = 0
                xsc = 0
                ysc = 0
                evc = 0
                NSB = CAP // 128

                def prep_loads(ei, e):
                    for q in range(NSB):
                        DMA(sp, xs[q][:], xg_dram[e * CAP + q * 128:e * CAP + (q + 1) * 128, :], None, writes=[B_xs[q]])

                def prep_transposes(ei, e):
                    xb_ = ei % 2
                    for q in range(NSB):
                        tb = TRBS[q % 2]
                        for c in range(8):
                            TR(trvs[q % 2][:, c, :], xs[q][:, c * 128:(c + 1) * 128], identb[:], [B_xs[q], B_identb], [BP[tb]], inc=(c == 7))
                        CP(act if q % 2 == 0 else dve, xgT[xb_][:, :, q * 128:(q + 1) * 128], trvs[q % 2], [BP[tb]], [B_xgT[xb_][q]])

                if experts[0] < NEXP:
                    prep_loads(0, experts[0])
                    prep_transposes(0, experts[0])
                for ei, e in enumerate(experts):
                    s_ = e % NWB
                    xb_ = ei % 2
                    nxt = experts[ei + 1] if ei + 1 < len(experts) else None
                    if nxt is not None:
                        load_expert(nxt)
                        if nxt < NEXP:
                            prep_loads(ei + 1, nxt)
                    nblk = 1 if e < NEXP else 4
                    W = CAP if e < NEXP else 512
                    for blk in range(nblk):
                        hb = uidx % 2
                        uidx += 1
                        for hc in range(2):
                            gu = []
                            for (wt, Bw) in ((wg, B_wg), (wu, B_wu)):
                                bk = GUB[guc[0] % 3]
                                guc[0] += 1
                                gu.append(bk)
                                for kc in range(8):
                                    if e < NEXP:
                                        rhs, Br = xgT[xb_][:, kc, :], B_xgT[xb_]
                                    else:
                                        rhs, Br = h1T[:, kc, blk * 512:(blk + 1) * 512], B_h1T[blk * 4:(blk + 1) * 4]
                                    MM(pb[bk][:, 0:W], wt[s_][:, kc, hc * 128:(hc + 1) * 128], rhs, kc == 0, kc == 7,
                                       [Bw[s_]] + Br, [BP[bk]], inc=(kc == 7))
                            A(sl[hc][:, 0:W], pb[gu[0]][:, 0:W], AF.Silu, [BP[gu[0]]], [B_sl[hc]])
                            TT(dve, hT[hb][:, hc, 0:W], sl[hc][:, 0:W], pb[gu[1]][:, 0:W], ALU.mult, [B_sl[hc], BP[gu[1]]], [B_hT[hb][hc]])
                        if blk == 0 and nxt is not None and nxt < NEXP:
                            prep_transposes(ei + 1, nxt)
                        for tl in range(W // 128):
                            if e < NEXP:
                                yi = ysc % 3
                                ysc += 1
                            for hf in range(2):
                                yb = YB[yc % 3]
                                yc += 1
                                for hc in range(2):
                                    MM(pb[yb][:], hT[hb][:, hc, tl * 128:(tl + 1) * 128], wd[s_][:, hc, hf * 512:(hf + 1) * 512], hc == 0, hc == 1,
                                       [B_hT[hb][hc], B_wd[s_]], [BP[yb]], inc=(hc == 1))
                                if e < NEXP:
                                    CP(act if evc % 2 == 0 else dve, ysb[yi][:, hf * 512:(hf + 1) * 512], pb[yb][:], [BP[yb]], [B_ysb[yi]])
                                    evc += 1
                                else:
                                    i = blk * 4 + tl
                                    acc = h1[:, i, hf * 512:(hf + 1) * 512]
                                    TT(dve, acc, pb[yb][:], acc, ALU.add, [BP[yb], B_h1[i][hf]], [B_h1[i][hf]])
                            if e < NEXP:
                                act.dma(lambda en, e=e, tl=tl, yi=yi: en.dma_start(out=y_dram[e * CAP + tl * 128:e * CAP + (tl + 1) * 128, :], in_=ysb[yi][:]),
                                        ds_yst[yi], reads=[B_ysb[yi]], writes=[])
                P.barrier()
                w8f = w8_all[:].rearrange("p t k -> p (t k)")
                w8hi = sl[0][:, 0:128]; B_w8hi = Buf("w8hi")
                w8lo = hT[0][:, 0, 0:128]; B_w8lo = Buf("w8lo")
                w8lf = ostg[0][:, 0:128]; B_w8lf = Buf("w8lf")
                CP(dve, w8hi, w8f, B_w8, [B_w8hi])
                TT(dve, w8lf, w8f, w8hi, ALU.subtract, B_w8 + [B_w8hi], [B_w8lf])
                CP(dve, w8lo, w8lf, [B_w8lf], [B_w8lo])
                Dm = [[wt[b_][:].rearrange("p c n -> p (c n)")[:, 0:1024].rearrange("p (k m) -> p k m", m=128) for wt in (wg, wu)] for b_ in range(2)]
                B_Dm = [[Buf(f"Dm{b_}_{j}") for j in range(2)] for b_ in range(2)]
                id3 = identb[:].rearrange("p (o m) -> p o m", o=1).to_broadcast([128, 8, 128])
                gk = 0
                def make_D(i):
                    bf_ = i % 2
                    for j, (wsrc, Bsrc) in enumerate(((w8hi, B_w8hi), (w8lo, B_w8lo))):
                        TT(dve, Dm[bf_][j], id3, wsrc[:, i * 8:(i + 1) * 8].rearrange("p (k o) -> p k o", o=1).to_broadcast([128, 8, 128]), ALU.mult,
                           [B_identb, Bsrc], [B_Dm[bf_][j]])

                make_D(0)
                for i in range(16):
                    bf_ = i % 2
                    for k in range(8):
                        r = gk % NYK
                        gk += 1
                        pool.dma(lambda en, i=i, k=k, r=r: en.indirect_dma_start(
                            out=yk[r][:], out_offset=None, in_=y_dram[:, :], in_offset=bass.IndirectOffsetOnAxis(ap=dest8[:, i * 8 + k:i * 8 + k + 1], axis=0),
                            bounds_check=P.bc_reg(en, NROWS - 1), oob_is_err=False), ds_pull[r], reads=[B_d8[i]], writes=[B_yk[r]])
                        for hf in range(2):
                            bk = 2 * bf_ + hf
                            MM(pb[bk][:], Dm[bf_][0][:, k, :], yk[r][:, hf * 512:(hf + 1) * 512], k == 0, False, [B_Dm[bf_][0], B_yk[r]], [BP[bk]], inc=False)
                            MM(pb[bk][:], Dm[bf_][1][:, k, :], yk[r][:, hf * 512:(hf + 1) * 512], False, k == 7, [B_Dm[bf_][1], B_yk[r]], [BP[bk]], inc=True)
                    if i + 1 < 16:
                        make_D(i + 1)
                    for hf in range(2):
                        acc = h1[:, i, hf * 512:(hf + 1) * 512]
                        TT(dve, acc, pb[2 * bf_ + hf][:], acc, ALU.add, [BP[2 * bf_ + hf], B_h1[i][hf]], [B_h1[i][hf]])
                    o = ostg[i % 2]
                    layer_norm(h1[:, i, :], B_h1[i], o[:], [B_ostg[i % 2]], eng_b=dve, on_act=True)
                    sp.dma(lambda en, i=i, o=o: en.dma_start(out=out[i * 128:(i + 1) * 128, :], in_=o[:]), ds_out[i % 2], reads=[B_ostg[i % 2]], writes=[])
                for d in ds_out:
                    sp.wait_tok(Tok(d.sem, d.count, None))
                P.emit()
    return nc


_CACHE = {}


def _consts(hf):
    identb = np.eye(128, dtype=np.float32).astype(ml_dtypes.bfloat16)
    identf = np.eye(128, dtype=np.float32)
    s = np.arange(64)[:, None]
    t = np.arange(64)[None, :]
    cmask = (t >= s).astype(np.float32)
    rmask = np.ones((128, 512), np.float32)
    rmask[:, ::64] = 0.0
    inv = np.zeros((128, 4, 16), np.float32)
    for g, w in enumerate(WINS):
        if hf == 0:
            inv[:, g, :] = 1.0 / np.minimum(np.arange(1, 17), w).astype(np.float32)
        else:
            inv[:, g, :] = 1.0 / w
    tp = np.arange(128)[:, None]
    tt = np.arange(128)[None, :]
    tri = (tp < tt).astype(np.float32).astype(ml_dtypes.bfloat16)
    iota = np.tile(np.arange(64, dtype=np.float32), (128, 1))
    ecoff = iota * float(CAP)
    return dict(c_identb=identb, c_identf=identf, c_cmask=cmask, c_rmask=rmask, c_invcnt=inv, c_tri=tri, c_iota=iota, c_ecoff=ecoff)


def make_in_maps(inputs, n_cores=8):
    x = np.asarray(inputs["x"], np.float32)
    sq = lambda k: np.ascontiguousarray(np.asarray(inputs[k], np.float32)[0])
    shared = {k: sq(k) for k in ["w_in", "hg_norm_g", "w_pool", "pool_scale", "w_out", "ln1_g", "ln1_b", "w_router", "router_bias",
                                 "w_gate", "w_up", "w_down", "ws_gate", "ws_up", "ws_down", "ln2_g", "ln2_b"]}
    shared["hg_lb_logits"] = np.ascontiguousarray(np.asarray(inputs["hg_lb_logits"], np.float32))
    in_maps = []
    for c in range(n_cores):
        b, hf = c // 2, c % 2
        xl = np.zeros((NLOC, D), np.float32)
        if hf == 1:
            xl[:NMAIN] = x[b, :NMAIN]
        xl[NMAIN:] = x[b, hf * NMAIN:(hf + 1) * NMAIN]
        m = dict(shared)
        m["xloc"] = xl
        m.update(_consts(hf))
        in_maps.append(m)
    return in_maps


def kernel(**inputs):
    if "nc" not in _CACHE:
        _CACHE["nc"] = build()
    nc = _CACHE["nc"]
    in_maps = make_in_maps(inputs)
    res = run_bass_kernel_spmd(nc, in_maps, core_ids=list(range(8)))
    x = inputs["x"]
    outp = np.zeros(x.shape, np.float32)
    for c in range(8):
        b, hf = c // 2, c % 2
        outp[b, hf * NMAIN:(hf + 1) * NMAIN] = res.results[c]["out"]
    return outp
```
